# Optimizing a Trainium2 kernel written in Bass

```python
import math
import jax, jax.numpy as jnp
from jax import lax
import numpy as np

D_MODEL = 2048
BATCH = 4
SEQ = 4096
DEPTH = 4

HEAD_DIM = 128
RET_HEADS = D_MODEL // 512
MLSTM_HEADS = D_MODEL // 512
FOX_HEADS = D_MODEL // 256
D_RET = RET_HEADS * HEAD_DIM
D_MLSTM = MLSTM_HEADS * HEAD_DIM
D_FOX = FOX_HEADS * HEAD_DIM
D_MIX = D_RET + D_MLSTM + D_FOX
CHUNK = 128
Q_BLOCK = 128
CONV_WIDTH = 4
ROPE_BASE = 10000.0
N_GROUPS = 4
EXPERTS_PER_GROUP = 8
N_EXPERTS = N_GROUPS * EXPERTS_PER_GROUP
TOP_K_IN_GROUP = 2
D_EXPERT = D_MODEL // 4
N_MOD = 6
EPS = 1e-6
SPLIT_SIZES = (D_RET, D_RET, D_RET, D_RET,
               D_MLSTM, D_MLSTM, D_MLSTM, MLSTM_HEADS, MLSTM_HEADS,
               D_FOX, D_FOX, D_FOX, FOX_HEADS)
N_IN = 4 * D_RET + 3 * D_MLSTM + 2 * MLSTM_HEADS + 3 * D_FOX + FOX_HEADS

kernel_name = "hybrid_ret_mlstm_fox_hmoe_adaln"


def rmsnorm(x, g):
    xf = x.astype(jnp.float32)
    y = xf * lax.rsqrt(jnp.mean(xf * xf, axis=-1, keepdims=True) + EPS)
    return (y * g.astype(jnp.float32)).astype(x.dtype)


def head_groupnorm(y, g, out_dtype):
    B, S, H, d = y.shape
    yf = y.astype(jnp.float32)
    mu = jnp.mean(yf, axis=-1, keepdims=True)
    var = jnp.mean(jnp.square(yf - mu), axis=-1, keepdims=True)
    yn = (yf - mu) * lax.rsqrt(var + EPS)
    return (yn * g.astype(jnp.float32).reshape(H, d)).astype(out_dtype).reshape(B, S, H * d)


def rotary(x, pos):
    half = x.shape[-1] // 2
    inv = ROPE_BASE ** (-jnp.arange(half, dtype=jnp.float32) / half)
    ang = pos[:, None] * inv[None, :]
    cos = jnp.cos(ang)[None, :, None, :]
    sin = jnp.sin(ang)[None, :, None, :]
    x1, x2 = x[..., :half], x[..., half:]
    return jnp.concatenate([x1 * cos - x2 * sin, x1 * sin + x2 * cos], axis=-1).astype(x.dtype)


def to_chunks(t):
    B, S, H, d = t.shape
    return t.reshape(B, S // CHUNK, CHUNK, H, d).transpose(0, 1, 3, 2, 4)


def from_chunks(t):
    B, nC, H, L, d = t.shape
    return t.transpose(0, 1, 3, 2, 4).reshape(B, nC * L, H, d)


def retention_chunkwise(q, k, v):
    H, d = q.shape[2], q.shape[3]
    log_gamma = jnp.log1p(-jnp.power(2.0, -5.0 - jnp.arange(H, dtype=jnp.float32)))
    pos = jnp.arange(CHUNK, dtype=jnp.float32)
    diff = pos[:, None] - pos[None, :]
    decay = jnp.where(diff >= 0, jnp.exp(jnp.maximum(diff, 0.0)[None] * log_gamma[:, None, None]), 0.0)
    zeta = jnp.exp((CHUNK - 1 - pos)[None, :] * log_gamma[:, None])
    xi = jnp.exp((pos + 1)[None, :] * log_gamma[:, None])
    gamma_chunk = jnp.exp(CHUNK * log_gamma)
    q_c, k_c, v_c = to_chunks(q), to_chunks(k * d ** -0.5), to_chunks(v)
    s = jnp.einsum('bchid,bchjd->bchij', q_c, k_c) * decay
    intra = jnp.einsum('bchij,bchje->bchie', s, v_c)
    kv = jnp.einsum('bchjd,hj,bchje->bchde', k_c, zeta, v_c)

    def step(R, kv_i):
        return R * gamma_chunk[None, :, None, None] + kv_i, R

    _, R_prev = lax.scan(step, jnp.zeros_like(kv[:, 0]), kv.swapaxes(0, 1))
    R_prev = R_prev.swapaxes(0, 1)
    cross = jnp.einsum('bchid,bchde->bchie', q_c, R_prev) * xi[None, None, :, :, None]
    return from_chunks(intra + cross)


def mlstm_chunkwise(q, k, v, i_pre, f_pre):
    B, S, H, d = q.shape
    nC = S // CHUNK
    q_c, k_c, v_c = to_chunks(q), to_chunks(k * d ** -0.5), to_chunks(v)

    def gate_chunks(t):
        return t.astype(jnp.float32).reshape(B, nC, CHUNK, H).transpose(0, 1, 3, 2)

    log_f = jax.nn.log_sigmoid(gate_chunks(f_pre))
    log_i = gate_chunks(i_pre)
    b = jnp.cumsum(log_f, axis=-1)
    b_end = b[..., -1]
    causal = jnp.tril(jnp.ones((CHUNK, CHUNK), dtype=bool))
    d_log = jnp.where(causal, b[..., :, None] - b[..., None, :] + log_i[..., None, :], -jnp.inf)
    a = b_end[..., None] - b + log_i
    a_max = jnp.max(a, axis=-1)
    w = jnp.exp(a - a_max[..., None])
    kv = jnp.einsum('bchj,bchjd,bchje->bchde', w, k_c, v_c)
    ks = jnp.einsum('bchj,bchjd->bchd', w, k_c)

    def step(carry, inp):
        C, n, m = carry
        kv_i, ks_i, amax_i, bend_i = inp
        m_new = jnp.maximum(bend_i + m, amax_i)
        s_old = jnp.exp(bend_i + m - m_new)
        s_new = jnp.exp(amax_i - m_new)
        C_new = s_old[..., None, None] * C + s_new[..., None, None] * kv_i
        n_new = s_old[..., None] * n + s_new[..., None] * ks_i
        return (C_new, n_new, m_new), (C, n, m)

    init = (jnp.zeros((B, H, d, d), kv.dtype), jnp.zeros((B, H, d), ks.dtype),
            jnp.full((B, H), -jnp.inf, jnp.float32))
    xs = (kv.swapaxes(0, 1), ks.swapaxes(0, 1), a_max.swapaxes(0, 1), b_end.swapaxes(0, 1))
    _, (C_prev, n_prev, m_prev) = lax.scan(step, init, xs)
    C_prev, n_prev, m_prev = C_prev.swapaxes(0, 1), n_prev.swapaxes(0, 1), m_prev.swapaxes(0, 1)
    inter_log = b + m_prev[..., None]
    m_t = jnp.maximum(jnp.max(d_log, axis=-1), inter_log)
    d_w = jnp.exp(d_log - m_t[..., None])
    inter_w = jnp.exp(inter_log - m_t)
    s = jnp.einsum('bchid,bchjd->bchij', q_c, k_c) * d_w
    num = (jnp.einsum('bchij,bchje->bchie', s, v_c)
           + inter_w[..., None] * jnp.einsum('bchid,bchde->bchie', q_c, C_prev))
    den = jnp.sum(s, axis=-1) + inter_w * jnp.einsum('bchid,bchd->bchi', q_c, n_prev)
    h = num / jnp.maximum(jnp.abs(den), jnp.exp(-m_t))[..., None]
    return from_chunks(h)


def forgetting_attention(q, k, v, f_pre):
    B, S, H, d = q.shape
    nQ = S // Q_BLOCK
    F = jnp.cumsum(jax.nn.log_sigmoid(f_pre.astype(jnp.float32)), axis=1).transpose(0, 2, 1)
    k_t = k.transpose(0, 2, 1, 3)
    v_t = v.transpose(0, 2, 1, 3)
    q_b = q.reshape(B, nQ, Q_BLOCK, H, d).transpose(1, 0, 3, 2, 4)
    F_b = F.reshape(B, H, nQ, Q_BLOCK).transpose(2, 0, 1, 3)
    k_pos = jnp.arange(S)
    scale = d ** -0.5

    def block(args):
        q_i, F_i, blk = args
        logits = (jnp.einsum('bhqd,bhkd->bhqk', q_i, k_t).astype(jnp.float32) * scale
                  + F_i[..., :, None] - F[:, :, None, :])
        q_pos = blk * Q_BLOCK + jnp.arange(Q_BLOCK)
        logits = jnp.where(k_pos[None, :] <= q_pos[:, None], logits, -jnp.inf)
        p = jax.nn.softmax(logits, axis=-1)
        return jnp.einsum('bhqk,bhkd->bhqd', p.astype(v_t.dtype), v_t)

    out = lax.map(block, (q_b, F_b, jnp.arange(nQ)))
    return out.transpose(1, 0, 3, 2, 4).reshape(B, S, H, d)


def causal_depthwise_conv(x, w, bias):
    y = lax.conv_general_dilated(x, w[:, None, :], window_strides=(1,),
                                 padding=[(CONV_WIDTH - 1, 0)],
                                 dimension_numbers=('NWC', 'WIO', 'NWC'),
                                 feature_group_count=x.shape[-1])
    return y + bias


def mixing_layer(h, w_in, ret_gn_g, conv_w, conv_b, m_wq, m_wk, m_i_b, m_f_b, m_gn_g, fox_f_b, w_out, pos):
    B, S, _ = h.shape
    proj = h @ w_in
    parts, o = [], 0
    for n in SPLIT_SIZES:
        parts.append(proj[..., o:o + n])
        o += n
    rq, rk, rv, rg, mx, mv, mo, mi, mf, fq, fk, fv, ff = parts

    def heads(t, H):
        return t.reshape(B, S, H, HEAD_DIM)

    y_ret = retention_chunkwise(rotary(heads(rq, RET_HEADS), pos), rotary(heads(rk, RET_HEADS), pos),
                                heads(rv, RET_HEADS))
    y_ret = head_groupnorm(y_ret, ret_gn_g, h.dtype) * jax.nn.silu(rg)

    xc = heads(jax.nn.silu(causal_depthwise_conv(mx, conv_w, conv_b)), MLSTM_HEADS)
    mq = jnp.einsum('bshd,hde->bshe', xc, m_wq)
    mk = jnp.einsum('bshd,hde->bshe', xc, m_wk)
    y_m = mlstm_chunkwise(mq, mk, heads(mv, MLSTM_HEADS), mi + m_i_b, mf + m_f_b)
    y_m = jax.nn.sigmoid(mo) * head_groupnorm(y_m, m_gn_g, h.dtype)

    y_f = forgetting_attention(heads(fq, FOX_HEADS), heads(fk, FOX_HEADS), heads(fv, FOX_HEADS),
                               ff + fox_f_b).reshape(B, S, D_FOX)

    y = jnp.concatenate([y_ret.astype(h.dtype), y_m.astype(h.dtype), y_f.astype(h.dtype)], axis=-1)
    return (y @ w_out).astype(h.dtype)


def hierarchical_moe(h, rg_w, rg_b, re_w, re_b, w1, w3, w2):
    B, S, D = h.shape
    t = h.reshape(-1, D)
    T = t.shape[0]
    g_prob = jax.nn.softmax((t @ rg_w + rg_b).astype(jnp.float32), axis=-1)
    g_val, g_idx = lax.top_k(g_prob, 1)
    e_logits = (t @ re_w + re_b).astype(jnp.float32).reshape(T, N_GROUPS, EXPERTS_PER_GROUP)
    e_sel = jnp.take_along_axis(e_logits, g_idx[:, :, None], axis=1)[:, 0]
    e_prob = jax.nn.softmax(e_sel, axis=-1)
    e_val, e_idx = lax.top_k(e_prob, TOP_K_IN_GROUP)
    e_val = e_val / jnp.sum(e_val, axis=-1, keepdims=True)
    gate = g_val * e_val
    expert_id = g_idx * EXPERTS_PER_GROUP + e_idx
    combine = jnp.sum(jax.nn.one_hot(expert_id, N_EXPERTS, dtype=jnp.float32) * gate[..., None], axis=1)
    combine = combine.astype(t.dtype)
    out = jnp.zeros_like(t)
    for e in range(N_EXPERTS):
        y_e = (jax.nn.silu(t @ w1[e]) * (t @ w3[e])) @ w2[e]
        out = out + combine[:, e:e + 1] * y_e
    return out.reshape(B, S, D)


def setup_inputs(seed: int = 0) -> dict:
    key = jax.random.key(seed)
    ks = jax.random.split(key, 26)

    def nrm(k, shape, scale):
        return scale * jax.random.normal(k, shape, jnp.float32)

    L, D = DEPTH, D_MODEL
    return {
        "x": nrm(ks[0], (BATCH, SEQ, D), 1.0),
        "c": nrm(ks[1], (BATCH, D), 1.0),
        "ada_w": nrm(ks[2], (L, D, N_MOD * D), 0.5 * D ** -0.5),
        "ada_b": nrm(ks[3], (L, N_MOD * D), 0.01),
        "norm1_g": 1.0 + nrm(ks[4], (L, D), 0.05),
        "w_in": nrm(ks[5], (L, D, N_IN), D ** -0.5),
        "ret_gn_g": 1.0 + nrm(ks[6], (L, D_RET), 0.05),
        "mlstm_conv_w": nrm(ks[7], (L, CONV_WIDTH, D_MLSTM), CONV_WIDTH ** -0.5),
        "mlstm_conv_b": nrm(ks[8], (L, D_MLSTM), 0.01),
        "mlstm_wq": nrm(ks[9], (L, MLSTM_HEADS, HEAD_DIM, HEAD_DIM), HEAD_DIM ** -0.5),
        "mlstm_wk": nrm(ks[10], (L, MLSTM_HEADS, HEAD_DIM, HEAD_DIM), HEAD_DIM ** -0.5),
        "mlstm_i_b": nrm(ks[11], (L, MLSTM_HEADS), 0.1),
        "mlstm_f_b": jnp.linspace(3.0, 6.0, MLSTM_HEADS, dtype=jnp.float32)[None, :] + nrm(ks[12], (L, MLSTM_HEADS), 0.1),
        "mlstm_gn_g": 1.0 + nrm(ks[13], (L, D_MLSTM), 0.05),
        "fox_f_b": jnp.linspace(2.0, 5.0, FOX_HEADS, dtype=jnp.float32)[None, :] + nrm(ks[14], (L, FOX_HEADS), 0.1),
        "w_out": nrm(ks[15], (L, D_MIX, D), D_MIX ** -0.5),
        "norm2_g": 1.0 + nrm(ks[16], (L, D), 0.05),
        "router_group_w": nrm(ks[17], (L, D, N_GROUPS), D ** -0.5),
        "router_group_b": nrm(ks[18], (L, N_GROUPS), 0.01),
        "router_expert_w": nrm(ks[19], (L, D, N_EXPERTS), D ** -0.5),
        "router_expert_b": nrm(ks[20], (L, N_EXPERTS), 0.01),
        "moe_w1": nrm(ks[21], (L, N_EXPERTS, D, D_EXPERT), D ** -0.5),
        "moe_w3": nrm(ks[22], (L, N_EXPERTS, D, D_EXPERT), D ** -0.5),
        "moe_w2": nrm(ks[23], (L, N_EXPERTS, D_EXPERT, D), D_EXPERT ** -0.5),
        "final_g": 1.0 + nrm(ks[24], (D,), 0.05),
    }


def reference(x, c, ada_w, ada_b, norm1_g, w_in, ret_gn_g, mlstm_conv_w, mlstm_conv_b, mlstm_wq, mlstm_wk,
              mlstm_i_b, mlstm_f_b, mlstm_gn_g, fox_f_b, w_out, norm2_g, router_group_w, router_group_b,
              router_expert_w, router_expert_b, moe_w1, moe_w3, moe_w2, final_g):
    S = x.shape[1]
    pos = jnp.arange(S, dtype=jnp.float32)
    c_act = jax.nn.silu(c)
    for l in range(DEPTH):
        mod = c_act @ ada_w[l] + ada_b[l]
        sh1, sc1, g1, sh2, sc2, g2 = jnp.split(mod[:, None, :], N_MOD, axis=-1)
        h = rmsnorm(x, norm1_g[l]) * (1.0 + sc1) + sh1
        x = x + g1 * mixing_layer(h, w_in[l], ret_gn_g[l], mlstm_conv_w[l], mlstm_conv_b[l], mlstm_wq[l],
                                  mlstm_wk[l], mlstm_i_b[l], mlstm_f_b[l], mlstm_gn_g[l], fox_f_b[l],
                                  w_out[l], pos)
        h = rmsnorm(x, norm2_g[l]) * (1.0 + sc2) + sh2
        x = x + g2 * hierarchical_moe(h, router_group_w[l], router_group_b[l], router_expert_w[l],
                                      router_expert_b[l], moe_w1[l], moe_w3[l], moe_w2[l])
    return rmsnorm(x, final_g)
```

```python
from contextlib import ExitStack
import numpy as np
import ml_dtypes
import concourse.bass as bass
import concourse.mybir as mybir
from concourse.bass_utils import run_bass_kernel_spmd

F32 = mybir.dt.float32
BF16 = mybir.dt.bfloat16
AF = mybir.ActivationFunctionType
ALU = mybir.AluOpType
AX = mybir.AxisListType

D = 2048
SEQ = 4096
NB = 4
DEPTH = 4
HD = 128
NCH = 32
KC = 16
TOKC = 2048
NEXP = 32
DEXP = 512
EPS = 1e-6
NEG = -1.0e30
ISQ = HD ** -0.5


class Buf:
    __slots__ = ("name", "w", "r")

    def __init__(self, name):
        self.name = name
        self.w = None
        self.r = {}


class Sched:
    NDMA = 8

    def __init__(self, nc):
        self.nc = nc
        self.eng = {"pe": nc.tensor, "dve": nc.vector, "act": nc.scalar,
                    "pool": nc.gpsimd, "sp": nc.sync}
        self.sem = {}
        self.cnt = {}
        self.waited = {}
        self._ctx = []
        for e in self.eng:
            g = nc.semaphore("s_" + e)
            self.sem[e] = g.__enter__()
            self._ctx.append(g)
            self.cnt[e] = 0
            self.waited[e] = {}
        self.dsem = {}
        self.dcnt = {}
        for q in ("sp", "act", "pool"):
            self.dsem[q] = []
            for i in range(self.NDMA):
                g = nc.semaphore("d_%s%d" % (q, i))
                self.dsem[q].append(g.__enter__())
                self._ctx.append(g)
            self.dcnt[q] = 0

    def close(self):
        for g in reversed(self._ctx):
            g.__exit__(None, None, None)

    def _wait(self, e, tok):
        if tok is None:
            return
        sem, val = tok
        if self.waited[e].get(sem.name, 0) >= val:
            return
        self.eng[e].wait_ge(sem, val)
        self.waited[e][sem.name] = val

    def _deps(self, e, reads, writes):
        for b in reads:
            self._wait(e, b.w)
        for b in writes:
            self._wait(e, b.w)
            for t in b.r.values():
                self._wait(e, t)

    def _mark(self, tok, reads, writes):
        for b in reads:
            b.r[tok[0].name] = tok
        for b in writes:
            b.w = tok
            b.r = {}

    def op(self, e, ins_fn, reads=(), writes=()):
        self._deps(e, reads, writes)
        ins = ins_fn()
        self.cnt[e] += 1
        ins.then_inc(self.sem[e], 1)
        tok = (self.sem[e], self.cnt[e])
        self._mark(tok, reads, writes)
        return tok

    def group(self, e, fns, reads=(), writes=()):
        self._deps(e, reads, writes)
        ins = None
        for f in fns:
            ins = f()
        self.cnt[e] += 1
        ins.then_inc(self.sem[e], 1)
        tok = (self.sem[e], self.cnt[e])
        self._mark(tok, reads, writes)
        return tok

    def dma(self, q, out, in_, reads=(), writes=()):
        i = self.dcnt[q]
        self.dcnt[q] += 1
        sem = self.dsem[q][i % self.NDMA]
        rnd = i // self.NDMA
        if rnd > 0:
            self._wait(q, (sem, 16 * rnd))
        self._deps(q, reads, writes)
        ins = self.eng[q].dma_start(out=out, in_=in_)
        ins.then_inc(sem, 16)
        tok = (sem, 16 * (rnd + 1))
        self._mark(tok, reads, writes)
        return tok

    def new_epoch(self):
        for e in ("pe", "dve", "act"):
            if self.cnt[e] == 0:
                continue
            g = self.nc.semaphore("s_%s_e%d" % (e, len(self._ctx)))
            self.sem[e] = g.__enter__()
            self._ctx.append(g)
            self.cnt[e] = 0

    def all_tokens(self):
        toks = []
        for e in self.eng:
            if self.cnt[e] > 0:
                toks.append((self.sem[e], self.cnt[e]))
        for q in self.dsem:
            n = self.dcnt[q]
            for j in range(self.NDMA):
                if n > j:
                    uses = (n - 1 - j) // self.NDMA + 1
                    toks.append((self.dsem[q][j], 16 * uses))
        return toks

    def barrier(self, engines=None):
        toks = self.all_tokens()
        for e in (engines or self.eng):
            for t in toks:
                self._wait(e, t)


class Ring:
    def __init__(self, tiles, name):
        self.tiles = tiles
        self.bufs = [Buf("%s%d" % (name, i)) for i in range(len(tiles))]
        self.i = 0

    def next(self):
        j = self.i % len(self.tiles)
        self.i += 1
        return self.tiles[j], self.bufs[j]


class Ctx:
    def __init__(self, nc):
        self.nc = nc
        self.S = Sched(nc)
        self.uid = 0

    def stack(self):
        return ExitStack()

    def sb(self, es, shape, dt, name=None):
        self.uid += 1
        return es.enter_context(self.nc.sbuf_tensor("%s_%d" % (name or "t", self.uid), list(shape), dt))

    def ps(self, es, shape, dt, name=None):
        self.uid += 1
        return es.enter_context(self.nc.psum_tensor("%s_%d" % (name or "p", self.uid), list(shape), dt))

    def sbring(self, es, n, shape, dt, name):
        return Ring([self.sb(es, shape, dt, name) for _ in range(n)], name)

    def psring(self, es, n, shape, dt, name):
        return Ring([self.ps(es, shape, dt, name) for _ in range(n)], name)


def emit_mod_cols(cx, es, adaw_src, cact, cactB, adab_col, out_col, out_B, ncols_chunks,
                  wring, psum, psB):
    nc, S = cx.nc, cx.S
    npieces = ncols_chunks // 4
    for pc in range(npieces):
        wt, wb = wring.next()
        S.dma("pool", wt[:], adaw_src[:, pc * 512:(pc + 1) * 512].rearrange("(k p) n -> p k n", p=128),
              writes=[wb])
        for c4 in range(4):
            cc = pc * 4 + c4
            fns = []
            for k in range(KC):
                fns.append(lambda k=k, c4=c4, cc=cc, wt=wt: nc.tensor.matmul(
                    psum[:, cc:cc + 1], wt[:, k, c4 * 128:(c4 + 1) * 128], cact[:, k, 0:1],
                    start=(k == 0), stop=(k == KC - 1)))
            S.group("pe", fns, reads=[wb, cactB], writes=[psB])
    S.op("dve", lambda: nc.vector.tensor_tensor(out=out_col[:], in0=psum[:, 0:ncols_chunks], in1=adab_col[:],
                                                op=ALU.add), reads=[psB], writes=[out_B])


def emit_cact(cx, es, c_rep_dram, cact, cactB, tmp):
    nc, S = cx.nc, cx.S
    tB = Buf("ctmp")
    S.dma("sp", tmp[:], c_rep_dram, writes=[tB])
    S.op("act", lambda: nc.scalar.activation(out=cact[:], in_=tmp[:], func=AF.Silu), reads=[tB], writes=[cactB])


def emit_rms_stats(cx, xt, xB, junk, junkB, ss, ssB, epsc):
    nc, S = cx.nc, cx.S
    S.op("act", lambda: nc.scalar.activation(out=junk[:], in_=xt[:], func=AF.Square, accum_out=ss[:, 0:1]),
         reads=[xB], writes=[junkB, ssB])
    S.op("act", lambda: nc.scalar.activation(out=ss[:, 1:2], in_=ss[:, 0:1], func=AF.Sqrt, scale=1.0 / D,
                                             bias=epsc[:, 0:1]), reads=[ssB], writes=[ssB])
    S.op("dve", lambda: nc.vector.reciprocal(out=ss[:, 2:3], in_=ss[:, 1:2]), reads=[ssB], writes=[ssB])


NFM = 18
NTM = 1536


def _ab_decl(nc, sfx=""):
    def din(name, shape, dt=F32):
        return nc.dram_tensor(name + sfx, list(shape), dt, kind="ExternalInput").ap()
    P = {}
    P["adaw"] = din("adaw", [D, 2 * D])
    P["adab_col"] = din("adab_col", [128, 32])
    P["g1col"] = din("g1col", [128, KC])
    P["w_fm"] = din("w_fm", [D, NFM * 128])
    P["w_tm"] = din("w_tm", [D, NTM])
    P["w_g"] = din("w_g", [D, 8])
    P["gn_rep"] = din("gn_rep", [128, 4, 128])
    P["conv_cols"] = din("conv_cols", [128, 2, 5])
    P["wq"] = din("wq", [2, 128, 128])
    P["wk"] = din("wk", [2, 128, 128])
    P["gate_b"] = din("gate_b", [4, 3])
    return P


def _ab_const_decl(nc):
    def din(name, shape, dt=F32):
        return nc.dram_tensor(name, list(shape), dt, kind="ExternalInput").ap()
    P = {}
    P["c_rep"] = din("c_rep", [128, KC, 128])
    P["ident_f"] = din("ident_f", [128, 128])
    P["rotC"] = din("rotC", [128, SEQ])
    P["rotS"] = din("rotS", [128, SEQ])
    P["ret_DT"] = din("ret_DT", [128, 2, 128])
    P["ret_cols"] = din("ret_cols", [128, 2, 3])
    P["rows_c"] = din("rows_c", [4, 3, SEQ])
    P["mask_m"] = din("mask_m", [128, 128])
    P["mask_d"] = din("mask_d", [128, 128])
    P["onehot"] = din("onehot", [4, 4, 128])
    return P


def _ab_scratch(nc):
    P = {}
    P["FMs"] = nc.dram_tensor("FMs", [NFM, 128, SEQ], BF16, kind="Internal").ap()
    P["TMs"] = nc.dram_tensor("TMs", [SEQ, NTM], BF16, kind="Internal").ap()
    P["Gs"] = nc.dram_tensor("Gs", [8, SEQ], F32, kind="Internal").ap()
    P["FMsB"], P["TMsB"], P["GsB"] = Buf("FMs"), Buf("TMs"), Buf("Gs")
    return P


def build_ab():
    nc = bass.Bass("TRN2", target_bir_lowering=False)
    cx = Ctx(nc)
    P = {}
    P.update(_ab_const_decl(nc))
    P.update(_ab_decl(nc))
    P.update(_ab_scratch(nc))
    x = nc.dram_tensor("x", [SEQ, D], F32, kind="ExternalInput").ap()
    y_out = nc.dram_tensor("y", [SEQ, 1024], BF16, kind="ExternalOutput").ap()
    yB = Buf("y")
    P["xsrc"] = lambda t0: x[t0:t0 + 128, :]
    P["xreads"] = []

    def ystore(S, col0, Yst, YB):
        S.dma("sp", y_out[:, col0:col0 + 128].rearrange("(c p) e -> p c e", p=128), Yst[:], reads=[YB], writes=[yB])
    P["ystore"] = ystore
    emit_ab(cx, P)
    cx.S.close()
    return nc


def emit_ab(cx, P):
    nc = cx.nc
    S = cx.S
    c_rep, adaw, adab_col, g1col = P["c_rep"], P["adaw"], P["adab_col"], P["g1col"]
    w_fm, w_tm, w_g, ident_f, rotC, rotS = P["w_fm"], P["w_tm"], P["w_g"], P["ident_f"], P["rotC"], P["rotS"]
    ret_DT, ret_cols, gn_rep, conv_cols = P["ret_DT"], P["ret_cols"], P["gn_rep"], P["conv_cols"]
    wq, wk, gate_b, rows_c, mask_m, mask_d, onehot = (P["wq"], P["wk"], P["gate_b"], P["rows_c"], P["mask_m"],
                                                      P["mask_d"], P["onehot"])
    FMs, TMs, Gs, FMsB, TMsB, GsB = P["FMs"], P["TMs"], P["Gs"], P["FMsB"], P["TMsB"], P["GsB"]

    with cx.stack() as es0:
        identf = cx.sb(es0, [128, 128], F32, "identf")
        identb = cx.sb(es0, [128, 128], BF16, "identb")
        epsc = cx.sb(es0, [128, 1], F32, "epsc")
        cB = Buf("consts")
        S.dma("sp", identf[:], ident_f, writes=[cB])
        S.op("dve", lambda: nc.vector.tensor_copy(out=identb[:], in_=identf[:]), reads=[cB], writes=[cB])
        S.op("dve", lambda: nc.vector.memset(epsc[:], EPS), writes=[cB])
        scale_col = cx.sb(es0, [128, KC], F32, "scalecol")
        shift_col = cx.sb(es0, [128, KC], F32, "shiftcol")
        modB = Buf("mod")

        with cx.stack() as es:
            cact = cx.sb(es, [128, KC, 128], BF16, "cact")
            cactB = Buf("cact")
            ctmp = cx.sb(es, [128, KC, 128], F32, "ctmp")
            emit_cact(cx, es, c_rep, cact, cactB, ctmp)
            wring = cx.sbring(es, 3, [128, KC, 512], BF16, "wp")
            modps = cx.ps(es, [128, 32], F32, "modps")
            modpsB = Buf("modps")
            modcol = cx.sb(es, [128, 32], F32, "modcol")
            adabc = cx.sb(es, [128, 32], F32, "adabc")
            g1c = cx.sb(es, [128, KC], F32, "g1c")
            aB = Buf("adab")
            S.dma("sp", adabc[:], adab_col, writes=[aB])
            S.dma("sp", g1c[:], g1col, writes=[aB])
            emit_mod_cols(cx, es, adaw, cact, cactB, adabc, modcol, modB, 32, wring, modps, modpsB)
            S.op("dve", lambda: nc.vector.tensor_copy(out=shift_col[:], in_=modcol[:, 0:KC]), reads=[modB], writes=[modB])
            S.op("dve", lambda: nc.vector.scalar_tensor_tensor(out=scale_col[:], in0=modcol[:, KC:2 * KC], scalar=1.0,
                                                               in1=g1c[:], op0=ALU.add, op1=ALU.mult),
                 reads=[modB, aB], writes=[modB])

            wg_t = cx.sb(es, [128, KC, 8], BF16, "wg")
            wgB = Buf("wg")
            S.dma("pool", wg_t[:], w_g.rearrange("(k p) n -> p k n", p=128), writes=[wgB])
            xring = cx.sbring(es, 2, [128, D], F32, "xt")
            xnring = cx.sbring(es, 2, [128, D], BF16, "xn")
            junk = cx.sb(es, [128, D], BF16, "junk")
            junkB = Buf("junk")
            ssring = cx.sbring(es, 2, [128, 4], F32, "ss")
            tpring = cx.psring(es, 2, [128, KC, 128], BF16, "tp")
            hring = cx.sbring(es, 2, [128, KC, 512], BF16, "hT")
            mmring = cx.psring(es, 3, [128, 512], F32, "mm")
            stg_b = cx.sbring(es, 3, [128, 512], BF16, "stgb")
            stg_f = cx.sbring(es, 2, [8, 512], F32, "stgf")
            pieces = [("fm", p * 512, min(512, NFM * 128 - p * 512)) for p in range((NFM * 128 + 511) // 512)]
            pieces += [("tm", p * 512, 512) for p in range(NTM // 512)]
            evac_i = 0
            for tb in range(SEQ // 512):
                hT, hB = hring.next()
                for tt in range(4):
                    t0 = tb * 512 + tt * 128
                    xt, xB = xring.next()
                    S.dma("sp", xt[:], P["xsrc"](t0), reads=P["xreads"], writes=[xB])
                    ss, ssB = ssring.next()
                    emit_rms_stats(cx, xt, xB, junk, junkB, ss, ssB, epsc)
                    xn, xnB = xnring.next()
                    S.op("dve", lambda: nc.vector.tensor_scalar(out=xn[:], in0=xt[:], scalar1=ss[:, 2:3], scalar2=None,
                                                                op0=ALU.mult), reads=[xB, ssB], writes=[xnB])
                    tp, tpB = tpring.next()
                    S.group("pe", [lambda k=k: nc.tensor.transpose(tp[:, k, :], xn[:, k * 128:(k + 1) * 128], identb[:])
                                   for k in range(KC)], reads=[xnB, cB], writes=[tpB])
                    for k in range(KC):
                        S.op("act", lambda k=k: nc.scalar.activation(
                            out=hT[:, k, tt * 128:(tt + 1) * 128], in_=tp[:, k, :], func=AF.Identity,
                            scale=scale_col[:, k:k + 1], bias=shift_col[:, k:k + 1]),
                             reads=[tpB, modB], writes=[hB])
                mm, mmB = mmring.next()
                S.group("pe", [lambda k=k: nc.tensor.matmul(mm[0:8, :], wg_t[:, k, :], hT[:, k, :],
                                                            start=(k == 0), stop=(k == KC - 1)) for k in range(KC)],
                        reads=[wgB, hB], writes=[mmB])
                sf, sfB = stg_f.next()
                S.op("dve", lambda: nc.vector.tensor_copy(out=sf[:], in_=mm[0:8, :]), reads=[mmB], writes=[sfB])
                S.dma("sp", Gs[:, tb * 512:(tb + 1) * 512], sf[:], reads=[sfB], writes=[GsB])
                for (kind, c0, ncol) in pieces:
                    wt, wb = wring.next()
                    src = w_fm if kind == "fm" else w_tm
                    S.dma("pool", wt[:, :, 0:ncol], src[:, c0:c0 + ncol].rearrange("(k p) n -> p k n", p=128),
                          writes=[wb])
                    if kind == "fm":
                        for c4 in range(ncol // 128):
                            ch = c0 // 128 + c4
                            mm, mmB = mmring.next()
                            S.group("pe", [lambda k=k, c4=c4, mm=mm, wt=wt: nc.tensor.matmul(
                                mm[:, :], wt[:, k, c4 * 128:(c4 + 1) * 128], hT[:, k, :],
                                start=(k == 0), stop=(k == KC - 1)) for k in range(KC)],
                                    reads=[wb, hB], writes=[mmB])
                            sg, sgB = stg_b.next()
                            eng = "act" if (evac_i % 2 == 0) else "dve"
                            evac_i += 1
                            if eng == "act":
                                S.op("act", lambda: nc.scalar.copy(out=sg[:], in_=mm[:, :]), reads=[mmB], writes=[sgB])
                            else:
                                S.op("dve", lambda: nc.vector.tensor_copy(out=sg[:], in_=mm[:, :]), reads=[mmB], writes=[sgB])
                            S.dma("sp", FMs[ch, :, tb * 512:(tb + 1) * 512], sg[:], reads=[sgB], writes=[FMsB])
                    else:
                        for tt in range(4):
                            mm, mmB = mmring.next()
                            S.group("pe", [lambda k=k, tt=tt, mm=mm, wt=wt: nc.tensor.matmul(
                                mm[:, :], hT[:, k, tt * 128:(tt + 1) * 128], wt[:, k, :],
                                start=(k == 0), stop=(k == KC - 1)) for k in range(KC)],
                                    reads=[wb, hB], writes=[mmB])
                            sg, sgB = stg_b.next()
                            eng = "act" if (evac_i % 2 == 0) else "dve"
                            evac_i += 1
                            if eng == "act":
                                S.op("act", lambda: nc.scalar.copy(out=sg[:], in_=mm[:, :]), reads=[mmB], writes=[sgB])
                            else:
                                S.op("dve", lambda: nc.vector.tensor_copy(out=sg[:], in_=mm[:, :]), reads=[mmB], writes=[sgB])
                            t0 = tb * 512 + tt * 128
                            S.dma("sp", TMs[t0:t0 + 128, c0:c0 + 512], sg[:], reads=[sgB], writes=[TMsB])
            S.barrier()

        es2 = es0
        tab = cx.sb(es2, [128, 4, NCH, NCH], F32, "tab")
        colsM = cx.sb(es2, [128, NCH, 8], F32, "colsM")
        sbM = cx.sb(es2, [128, 2, 64], F32, "sbM")
        tabB, colsB, sbB = Buf("tab"), Buf("colsM"), Buf("sbM")
        with cx.stack() as es:
            rows = cx.sb(es, [4, 3, SEQ], F32, "rows")
            gb = cx.sb(es, [4, 3], F32, "gb")
            ngb = cx.sb(es, [4, 3], F32, "ngb")
            one4 = cx.sb(es, [4, 1], F32, "one4")
            oh = cx.sb(es, [4, 4, 128], F32, "oh")
            rB = Buf("rows")
            S.dma("sp", rows[:], rows_c, writes=[rB])
            S.dma("sp", gb[:], gate_b, writes=[rB])
            S.dma("sp", oh[:], onehot, writes=[rB])
            S.op("dve", lambda: nc.vector.tensor_scalar(out=ngb[:], in0=gb[:], scalar1=-1.0, scalar2=None, op0=ALU.mult),
                 reads=[rB], writes=[rB])
            S.op("dve", lambda: nc.vector.memset(one4[:], 1.0), writes=[rB])
            pt = cx.ps(es, [128, NCH, 8], F32, "pt")
            ptB = Buf("pt")
            pb = cx.ps(es, [128, 4, 64], F32, "pb")
            pbB = Buf("pb")
            wB = Buf("work")
            with cx.stack() as esf:
                gff = cx.sb(esf, [4, SEQ], F32, "gff")
                A = cx.sb(esf, [4, SEQ], F32, "A")
                gB = Buf("g")
                S.dma("sp", gff[:], Gs[4:8, :], reads=[GsB], writes=[gB])
                S.op("act", lambda: nc.scalar.activation(out=gff[:], in_=gff[:], func=AF.Exp, scale=-1.0, bias=ngb[:, 2:3]),
                     reads=[gB, rB], writes=[gB])
                S.op("act", lambda: nc.scalar.activation(out=gff[:], in_=gff[:], func=AF.Ln, scale=1.0, bias=one4[:, 0:1]),
                     reads=[gB, rB], writes=[gB])
                S.op("dve", lambda: nc.vector.tensor_tensor_scan(out=A[:], data0=rows[:, 0, :], data1=gff[:], initial=0.0,
                                                                 op0=ALU.mult, op1=ALU.add), reads=[gB, rB], writes=[wB])
                S.group("pe", [lambda c=c: nc.tensor.transpose(pt[:, c, 0:4], A[:, c * 128:(c + 1) * 128], identf[0:4, 0:4])
                               for c in range(NCH)], reads=[wB, cB], writes=[ptB])
                AT = cx.sb(esf, [128, NCH, 4], F32, "AT")
                AendB_t = cx.sb(esf, [128, 4, NCH], F32, "AendB")
                S.op("dve", lambda: nc.vector.tensor_copy(out=AT[:], in_=pt[:, :, 0:4]), reads=[ptB], writes=[wB])
                Aend = cx.sb(esf, [4, NCH], F32, "Aend")
                S.op("dve", lambda: nc.vector.tensor_copy(out=Aend[:], in_=A[:].rearrange("h (c l) -> h c l", l=128)[:, :, 127]),
                     reads=[wB], writes=[wB])
                S.group("pe", [lambda h=h: nc.tensor.matmul(pb[:, h, 0:NCH], oh[:, h, :], Aend[:], start=True, stop=True)
                               for h in range(4)], reads=[wB, rB], writes=[pbB])
                S.op("dve", lambda: nc.vector.tensor_copy(out=AendB_t[:], in_=pb[:, :, 0:NCH]), reads=[pbB], writes=[wB])
                for h in range(4):
                    for qb in range(NCH):
                        S.op("dve", lambda h=h, qb=qb: nc.vector.tensor_scalar(
                            out=tab[:, h, qb, :], in0=AT[:, :, h], scalar1=AendB_t[:, h, qb:qb + 1], scalar2=None,
                            op0=ALU.subtract), reads=[wB], writes=[tabB])
                S.barrier()
            with cx.stack() as esm:
                g = cx.sb(esm, [2, SEQ], F32, "g")
                Lf = cx.sb(esm, [2, SEQ], F32, "Lf")
                Bc = cx.sb(esm, [2, SEQ], F32, "Bc")
                M = cx.sb(esm, [2, SEQ], F32, "M")
                mm_ = cx.sb(esm, [2, SEQ], F32, "mm")
                sm = cx.sb(esm, [2, 8, NCH], F32, "sm")
                mB = Buf("mwork")
                S.dma("sp", g[:], Gs[0:2, :], reads=[GsB], writes=[mB])
                S.dma("sp", Lf[:], Gs[2:4, :], reads=[GsB], writes=[mB])
                S.op("act", lambda: nc.scalar.activation(out=Lf[:], in_=Lf[:], func=AF.Exp, scale=-1.0, bias=ngb[0:2, 1:2]),
                     reads=[mB, rB], writes=[mB])
                S.op("act", lambda: nc.scalar.activation(out=Lf[:], in_=Lf[:], func=AF.Ln, scale=1.0, bias=one4[0:2, 0:1]),
                     reads=[mB, rB], writes=[mB])
                S.op("dve", lambda: nc.vector.tensor_tensor_scan(out=Bc[:], data0=rows[0:2, 1, :], data1=Lf[:], initial=0.0,
                                                                 op0=ALU.mult, op1=ALU.add), reads=[mB, rB], writes=[mB])
                S.op("dve", lambda: nc.vector.scalar_tensor_tensor(out=g[:], in0=g[:], scalar=gb[0:2, 0:1], in1=Bc[:],
                                                                   op0=ALU.add, op1=ALU.add), reads=[mB, rB], writes=[mB])
                S.op("dve", lambda: nc.vector.tensor_tensor_scan(out=M[:], data0=rows[0:2, 2, :], data1=g[:], initial=NEG,
                                                                 op0=ALU.add, op1=ALU.max), reads=[mB, rB], writes=[mB])
                Mv = M[:].rearrange("h (c l) -> h c l", l=128)
                Bv = Bc[:].rearrange("h (c l) -> h c l", l=128)
                S.op("dve", lambda: nc.vector.tensor_copy(out=sm[:, 0, :], in_=Mv[:, :, 127]), reads=[mB], writes=[mB])
                S.op("dve", lambda: nc.vector.tensor_copy(out=sm[:, 1, :], in_=Bv[:, :, 127]), reads=[mB], writes=[mB])
                S.op("dve", lambda: nc.vector.tensor_tensor_scan(out=sm[:, 2, :], data0=sm[:, 0, :], data1=sm[:, 1, :],
                                                                 initial=NEG, op0=ALU.max, op1=ALU.subtract),
                     reads=[mB], writes=[mB])
                S.op("dve", lambda: nc.vector.memset(sm[:, 3, 0:1], NEG), reads=[mB], writes=[mB])
                S.op("dve", lambda: nc.vector.tensor_copy(out=sm[:, 3, 1:NCH], in_=sm[:, 2, 0:NCH - 1]), reads=[mB], writes=[mB])
                S.op("dve", lambda: nc.vector.tensor_tensor(out=sm[:, 4, :], in0=sm[:, 3, :], in1=sm[:, 0, :], op=ALU.max),
                     reads=[mB], writes=[mB])
                S.op("dve", lambda: nc.vector.tensor_tensor(out=sm[:, 5, :], in0=sm[:, 3, :], in1=sm[:, 4, :], op=ALU.subtract),
                     reads=[mB], writes=[mB])
                S.op("dve", lambda: nc.vector.tensor_tensor(out=sm[:, 6, :], in0=sm[:, 0, :], in1=sm[:, 4, :], op=ALU.subtract),
                     reads=[mB], writes=[mB])
                S.op("act", lambda: nc.scalar.activation(out=sm[:, 5:7, :], in_=sm[:, 5:7, :], func=AF.Exp),
                     reads=[mB], writes=[mB])
                for c in range(NCH):
                    sl = slice(c * 128, (c + 1) * 128)
                    S.op("dve", lambda c=c, sl=sl: nc.vector.tensor_scalar(out=mm_[:, sl], in0=M[:, sl], scalar1=sm[:, 3, c:c + 1],
                                                                           scalar2=None, op0=ALU.max), reads=[mB], writes=[mB])
                    S.op("dve", lambda c=c, sl=sl: nc.vector.tensor_scalar(out=g[:, sl], in0=g[:, sl], scalar1=sm[:, 0, c:c + 1],
                                                                           scalar2=None, op0=ALU.subtract), reads=[mB], writes=[mB])
                    S.op("dve", lambda c=c, sl=sl: nc.vector.tensor_scalar(out=M[:, sl], in0=mm_[:, sl], scalar1=sm[:, 0, c:c + 1],
                                                                           scalar2=-1.0, op0=ALU.subtract, op1=ALU.mult),
                         reads=[mB], writes=[mB])
                S.op("dve", lambda: nc.vector.tensor_tensor(out=Bc[:], in0=Bc[:], in1=mm_[:], op=ALU.subtract),
                     reads=[mB], writes=[mB])
                for c in range(NCH):
                    sl = slice(c * 128, (c + 1) * 128)
                    S.op("dve", lambda c=c, sl=sl: nc.vector.tensor_scalar(out=mm_[:, sl], in0=mm_[:, sl], scalar1=sm[:, 3, c:c + 1],
                                                                           scalar2=-1.0, op0=ALU.subtract, op1=ALU.mult),
                         reads=[mB], writes=[mB])
                qs = [g, M, mm_, Bc]
                for qt in qs:
                    S.op("act", lambda qt=qt: nc.scalar.activation(out=qt[:], in_=qt[:], func=AF.Exp), reads=[mB], writes=[mB])
                fns = []
                for c in range(NCH):
                    for q in range(4):
                        fns.append(lambda c=c, q=q: nc.tensor.transpose(pt[:, c, q * 2:q * 2 + 2], qs[q][:, c * 128:(c + 1) * 128],
                                                                        identf[0:2, 0:2]))
                S.group("pe", fns, reads=[mB, cB, wB], writes=[ptB])
                S.op("dve", lambda: nc.vector.tensor_copy(out=colsM[:], in_=pt[:]), reads=[ptB], writes=[colsB])
                S.group("pe", [lambda h=h: nc.tensor.matmul(pb[:, h, :], oh[0:2, h, :],
                                                            sm[:, 5:7, :].rearrange("h a c -> h (a c)"), start=True, stop=True)
                               for h in range(2)], reads=[mB, rB, wB], writes=[pbB])
                S.op("dve", lambda: nc.vector.tensor_copy(out=sbM[:], in_=pb[:, 0:2, :]), reads=[pbB], writes=[sbB])
                S.barrier()

        with cx.stack() as es:
            QT = cx.sb(es, [128, SEQ], BF16, "QT")
            KT = cx.sb(es, [128, SEQ], BF16, "KT")
            Ktok = cx.sb(es, [128, NCH, 128], BF16, "Ktok")
            V1 = cx.sb(es, [128, NCH, 129], BF16, "V1")
            GT = cx.sb(es, [128, NCH, 128], BF16, "GT")
            Yst = cx.sb(es, [128, NCH, 128], BF16, "Yst")
            gnr = cx.sb(es, [128, 4, 128], F32, "gnr")
            f32a = cx.sb(es, [128, SEQ], F32, "f32a")
            f32b = cx.sb(es, [128, SEQ], F32, "f32b")
            ldq = cx.sb(es, [128, SEQ], BF16, "ldq")
            ldp = cx.sb(es, [128, SEQ], BF16, "ldp")
            rc = cx.sb(es, [128, SEQ], F32, "rotC")
            rs = cx.sb(es, [128, SEQ], F32, "rotS")
            rDT = cx.sb(es, [128, 2, 128], F32, "rDT")
            rcols = cx.sb(es, [128, 2, 3], F32, "rcols")
            ccols = cx.sb(es, [128, 2, 5], F32, "ccols")
            wqk = cx.sb(es, [128, 4, 128], BF16, "wqk")
            mskm = cx.sb(es, [128, 128], F32, "mskm")
            Rst = cx.sb(es, [128, 129], F32, "Rst")
            Rbf = cx.sb(es, [128, 129], BF16, "Rbf")
            kB = Buf("hconst")
            S.dma("sp", gnr[:], gn_rep, writes=[kB])
            S.dma("sp", rc[:], rotC, writes=[kB])
            S.dma("sp", rs[:], rotS, writes=[kB])
            S.dma("sp", rDT[:], ret_DT, writes=[kB])
            S.dma("sp", rcols[:], ret_cols, writes=[kB])
            S.dma("sp", ccols[:], conv_cols, writes=[kB])
            S.dma("sp", mskm[:], mask_m, writes=[kB])
            for i in range(2):
                S.dma("pool", wqk[:, i, :], wq[i], writes=[kB])
                S.dma("pool", wqk[:, 2 + i, :], wk[i], writes=[kB])
            QB, KB_, KtB, VB, GB, YB, RB = Buf("QT"), Buf("KT"), Buf("Ktok"), Buf("V1"), Buf("GT"), Buf("Yst"), Buf("R")
            lB, fB = Buf("ld"), Buf("f32")
            ps_s = cx.psring(es, 2, [128, 128], F32, "ps_s")
            ps_o = cx.psring(es, 1, [128, 256], F32, "ps_o")
            ps_x = cx.psring(es, 1, [128, 256], F32, "ps_x")
            ps_kv = cx.psring(es, 1, [128, 256], F32, "ps_kv")
            ps_t = cx.psring(es, 1, [128, 512], BF16, "ps_t")
            psq_ring = cx.psring(es, 2, [128, 512], F32, "psq")
            PTr = cx.sbring(es, 2, [128, 128], BF16, "PT")
            Vzr = cx.sbring(es, 2, [128, 129], BF16, "Vz")
            t1r = cx.sbring(es, 2, [128, 129], F32, "t1")
            t2r = cx.sbring(es, 2, [128, 129], F32, "t2")
            Rbr = cx.sbring(es, 2, [128, 129], BF16, "Rbr")
            ndr = cx.sbring(es, 2, [128, 132], F32, "nd")
            str_ = cx.sbring(es, 2, [128, 8], F32, "st")
            hnr = cx.sbring(es, 2, [128, 128], F32, "hn")
            sgr = cx.sbring(es, 2, [128, 128], F32, "sg")
            bnr = cx.sbring(es, 2, [128, 8], F32, "bn")

            def load_tok(dst, dstB, col0, width=128):
                S.dma("sp", dst[:, :, 0:width], TMs[:, col0:col0 + width].rearrange("(c p) e -> p c e", p=128),
                      reads=[TMsB], writes=[dstB])

            def groupnorm_gate(o_ap, oB, gate_func, gi_idx, c):
                bn, bnB = bnr.next()
                S.op("dve", lambda: nc.vector.bn_stats(out=bn[:, 0:6], in_=o_ap), reads=[oB], writes=[bnB])
                S.op("dve", lambda: nc.vector.bn_aggr(out=bn[:, 6:8], in_=bn[:, 0:6]), reads=[bnB], writes=[bnB])
                st, stB = str_.next()
                S.op("act", lambda: nc.scalar.activation(out=st[:, 0:1], in_=bn[:, 7:8], func=AF.Sqrt, scale=1.0,
                                                         bias=epsc[:, 0:1]), reads=[bnB, cB], writes=[stB])
                S.op("dve", lambda: nc.vector.reciprocal(out=st[:, 1:2], in_=st[:, 0:1]), reads=[stB], writes=[stB])
                hn, hnB = hnr.next()
                S.op("dve", lambda: nc.vector.tensor_scalar(out=hn[:], in0=o_ap, scalar1=bn[:, 6:7], scalar2=st[:, 1:2],
                                                            op0=ALU.subtract, op1=ALU.mult), reads=[oB, bnB, stB], writes=[hnB])
                sg, sgB = sgr.next()
                S.op("act", lambda: nc.scalar.activation(out=sg[:], in_=GT[:, c, :], func=gate_func), reads=[GB], writes=[sgB])
                S.op("dve", lambda: nc.vector.tensor_tensor(out=hn[:], in0=hn[:], in1=gnr[:, gi_idx, :], op=ALU.mult),
                     reads=[hnB, kB], writes=[hnB])
                S.op("dve", lambda: nc.vector.tensor_tensor(out=Yst[:, c, :], in0=hn[:], in1=sg[:], op=ALU.mult),
                     reads=[hnB, sgB], writes=[YB])

            for hh in range(2):
                for (dst, dstB, ch, chp) in ((QT, QB, 8 + hh, 12 + hh), (KT, KB_, 10 + hh, 14 + hh)):
                    S.dma("sp", ldq[:], FMs[ch], reads=[FMsB], writes=[lB])
                    S.dma("sp", ldp[:], FMs[chp], reads=[FMsB], writes=[lB])
                    S.op("dve", lambda: nc.vector.tensor_tensor(out=f32a[:], in0=ldq[:], in1=rc[:], op=ALU.mult),
                         reads=[lB, kB], writes=[fB])
                    S.op("dve", lambda: nc.vector.tensor_tensor(out=f32b[:], in0=ldp[:], in1=rs[:], op=ALU.mult),
                         reads=[lB, kB], writes=[fB])
                    S.op("dve", lambda dst=dst: nc.vector.tensor_tensor(out=dst[:], in0=f32a[:], in1=f32b[:], op=ALU.add),
                         reads=[fB], writes=[dstB])
                for c4 in range(NCH // 4):
                    pt_, ptB_ = ps_t.next()
                    S.group("pe", [lambda j=j, c4=c4: nc.tensor.transpose(pt_[:, j * 128:(j + 1) * 128],
                                                                          KT[:, (c4 * 4 + j) * 128:(c4 * 4 + j + 1) * 128], identb[:])
                                   for j in range(4)], reads=[KB_, cB], writes=[ptB_])
                    S.op("act", lambda c4=c4: nc.scalar.copy(out=Ktok[:, c4 * 4:(c4 + 1) * 4, :],
                                                             in_=pt_[:, :].rearrange("p (j e) -> p j e", e=128)),
                         reads=[ptB_], writes=[KtB])
                load_tok(V1, VB, hh * 128)
                load_tok(GT, GB, 256 + hh * 128)
                S.op("dve", lambda: nc.vector.memset(Rst[:], 0.0), writes=[RB])
                rb0, rb0B = Rbr.next()
                S.op("dve", lambda: nc.vector.memset(rb0[:], 0.0), writes=[rb0B])
                rcur = [rb0, rb0B]

                def ret_A(c):
                    sl = slice(c * 128, (c + 1) * 128)
                    pss, pssB = ps_s.next()
                    S.op("pe", lambda: nc.tensor.matmul(pss[:], KT[:, sl], QT[:, sl], start=True, stop=True),
                         reads=[KB_, QB], writes=[pssB])
                    PT, PTB = PTr.next()
                    S.op("dve", lambda: nc.vector.tensor_tensor(out=PT[:], in0=pss[:], in1=rDT[:, hh, :], op=ALU.mult),
                         reads=[pssB, kB], writes=[PTB])
                    pso, psoB = ps_o.next()
                    S.op("pe", lambda: nc.tensor.matmul(pso[:, 0:128], PT[:], V1[:, c, 0:128], start=True, stop=True),
                         reads=[PTB, VB], writes=[psoB])
                    t1, t1B = t1r.next()
                    S.op("act", lambda: nc.scalar.copy(out=t1[:, 0:128], in_=pso[:, 0:128]), reads=[psoB], writes=[t1B])
                    Vz, VzB = Vzr.next()
                    S.op("dve", lambda: nc.vector.tensor_scalar(out=Vz[:, 0:128], in0=V1[:, c, 0:128], scalar1=rcols[:, hh, 0:1],
                                                                scalar2=None, op0=ALU.mult), reads=[VB, kB], writes=[VzB])
                    pkv, pkvB = ps_kv.next()
                    S.op("pe", lambda: nc.tensor.matmul(pkv[:, 0:128], Ktok[:, c, :], Vz[:, 0:128], start=True, stop=True),
                         reads=[KtB, VzB], writes=[pkvB])
                    t2, t2B = t2r.next()
                    S.op("act", lambda: nc.scalar.copy(out=t2[:, 0:128], in_=pkv[:, 0:128]), reads=[pkvB], writes=[t2B])
                    return (t1, t1B, t2, t2B)

                def ret_C(c, hA):
                    sl = slice(c * 128, (c + 1) * 128)
                    t1, t1B, t2, t2B = hA
                    rb, rbB = rcur
                    psx, psxB = ps_x.next()
                    S.op("pe", lambda: nc.tensor.matmul(psx[:, 0:128], QT[:, sl], rb[:, 0:128], start=True, stop=True),
                         reads=[QB, rbB], writes=[psxB])
                    S.op("dve", lambda: nc.vector.scalar_tensor_tensor(out=Rst[:, 0:128], in0=Rst[:, 0:128],
                                                                       scalar=rcols[:, hh, 2:3], in1=t2[:, 0:128],
                                                                       op0=ALU.mult, op1=ALU.add),
                         reads=[t2B, kB, RB], writes=[RB])
                    rn, rnB = Rbr.next()
                    S.op("act", lambda: nc.scalar.copy(out=rn[:, 0:128], in_=Rst[:, 0:128]), reads=[RB], writes=[rnB])
                    rcur[0], rcur[1] = rn, rnB
                    nd, ndB = ndr.next()
                    S.op("dve", lambda: nc.vector.scalar_tensor_tensor(out=nd[:, 0:128], in0=psx[:, 0:128],
                                                                       scalar=rcols[:, hh, 1:2], in1=t1[:, 0:128],
                                                                       op0=ALU.mult, op1=ALU.add),
                         reads=[psxB, t1B, kB], writes=[ndB])
                    groupnorm_gate(nd[:, 0:128], ndB, AF.Silu, hh, c)
                hA = ret_A(0)
                for c in range(NCH):
                    hN = ret_A(c + 1) if c + 1 < NCH else None
                    ret_C(c, hA)
                    hA = hN
                P["ystore"](S, hh * 128, Yst, YB)

            S.op("dve", lambda: nc.vector.memset(V1[:, :, 128:129], 1.0), reads=[VB], writes=[VB])
            for hh in range(2):
                S.dma("sp", ldq[:], FMs[16 + hh], reads=[FMsB], writes=[lB])
                S.op("dve", lambda: nc.vector.tensor_scalar(out=f32a[:], in0=ldq[:], scalar1=ccols[:, hh, 3:4],
                                                            scalar2=ccols[:, hh, 4:5], op0=ALU.mult, op1=ALU.add),
                     reads=[lB, kB], writes=[fB])
                for j in range(3):
                    s = 3 - j
                    S.op("dve", lambda j=j, s=s: nc.vector.scalar_tensor_tensor(
                        out=f32a[:, s:SEQ], in0=ldq[:, 0:SEQ - s], scalar=ccols[:, hh, j:j + 1], in1=f32a[:, s:SEQ],
                        op0=ALU.mult, op1=ALU.add), reads=[lB, kB, fB], writes=[fB])
                S.op("act", lambda: nc.scalar.activation(out=ldp[:], in_=f32a[:], func=AF.Silu), reads=[fB], writes=[lB])
                for tb in range(SEQ // 512):
                    tsl = slice(tb * 512, (tb + 1) * 512)
                    for (dst, dstB, wi) in ((QT, QB, hh), (KT, KB_, 2 + hh)):
                        pq, pqB = psq_ring.next()
                        S.op("pe", lambda pq=pq, wi=wi, tsl=tsl: nc.tensor.matmul(pq[:], wqk[:, wi, :], ldp[:, tsl],
                                                                                  start=True, stop=True),
                             reads=[kB, lB], writes=[pqB])
                        S.op("act", lambda pq=pq, dst=dst, tsl=tsl: nc.scalar.copy(out=dst[:, tsl], in_=pq[:]),
                             reads=[pqB], writes=[dstB])
                    pq, pqB = psq_ring.next()
                    S.group("pe", [lambda j=j, pq=pq, tb=tb: nc.tensor.matmul(
                        pq[:, j * 128:(j + 1) * 128], ldp[:, (tb * 4 + j) * 128:(tb * 4 + j + 1) * 128], wqk[:, 2 + hh, :],
                        start=True, stop=True) for j in range(4)], reads=[kB, lB], writes=[pqB])
                    S.op("dve", lambda pq=pq, tb=tb: nc.vector.tensor_copy(
                        out=Ktok[:, tb * 4:(tb + 1) * 4, :], in_=pq[:].rearrange("p (j e) -> p j e", e=128)),
                         reads=[pqB], writes=[KtB])
                load_tok(V1, VB, 512 + hh * 128)
                load_tok(GT, GB, 768 + hh * 128)
                S.op("dve", lambda: nc.vector.memset(Rst[:], 0.0), reads=[RB], writes=[RB])
                rb0, rb0B = Rbr.next()
                S.op("dve", lambda: nc.vector.memset(rb0[:], 0.0), writes=[rb0B])
                rcur = [rb0, rb0B]

                def ml_A(c):
                    sl = slice(c * 128, (c + 1) * 128)

                    def col(q):
                        return colsM[:, c, q * 2 + hh:q * 2 + hh + 1]
                    pss, pssB = ps_s.next()
                    S.op("pe", lambda: nc.tensor.matmul(pss[:], KT[:, sl], QT[:, sl], start=True, stop=True),
                         reads=[KB_, QB], writes=[pssB])
                    PT, PTB = PTr.next()
                    S.op("dve", lambda: nc.vector.scalar_tensor_tensor(out=PT[:], in0=pss[:], scalar=col(0), in1=mskm[:],
                                                                       op0=ALU.mult, op1=ALU.mult),
                         reads=[pssB, kB, colsB], writes=[PTB])
                    pso, psoB = ps_o.next()
                    S.op("pe", lambda: nc.tensor.matmul(pso[:, 0:129], PT[:], V1[:, c, :], start=True, stop=True),
                         reads=[PTB, VB], writes=[psoB])
                    t1, t1B = t1r.next()
                    S.op("act", lambda: nc.scalar.activation(out=t1[:], in_=pso[:, 0:129], func=AF.Copy, scale=col(1)),
                         reads=[psoB, colsB], writes=[t1B])
                    Vz, VzB = Vzr.next()
                    S.op("dve", lambda: nc.vector.tensor_scalar(out=Vz[:], in0=V1[:, c, :], scalar1=col(0), scalar2=ISQ,
                                                                op0=ALU.mult, op1=ALU.mult), reads=[VB, colsB], writes=[VzB])
                    pkv, pkvB = ps_kv.next()
                    S.op("pe", lambda: nc.tensor.matmul(pkv[:, 0:129], Ktok[:, c, :], Vz[:], start=True, stop=True),
                         reads=[KtB, VzB], writes=[pkvB])
                    t2, t2B = t2r.next()
                    S.op("act", lambda: nc.scalar.activation(out=t2[:], in_=pkv[:, 0:129], func=AF.Copy,
                                                             scale=sbM[:, hh, 32 + c:33 + c]), reads=[pkvB, sbB], writes=[t2B])
                    return (t1, t1B, t2, t2B)

                def ml_C(c, hA):
                    sl = slice(c * 128, (c + 1) * 128)

                    def col(q):
                        return colsM[:, c, q * 2 + hh:q * 2 + hh + 1]
                    t1, t1B, t2, t2B = hA
                    rb, rbB = rcur
                    psx, psxB = ps_x.next()
                    S.op("pe", lambda: nc.tensor.matmul(psx[:, 0:129], QT[:, sl], rb[:], start=True, stop=True),
                         reads=[QB, rbB], writes=[psxB])
                    S.op("dve", lambda: nc.vector.scalar_tensor_tensor(out=Rst[:], in0=Rst[:], scalar=sbM[:, hh, c:c + 1],
                                                                       in1=t2[:], op0=ALU.mult, op1=ALU.add),
                         reads=[t2B, sbB, RB], writes=[RB])
                    rn, rnB = Rbr.next()
                    S.op("act", lambda: nc.scalar.copy(out=rn[:], in_=Rst[:]), reads=[RB], writes=[rnB])
                    rcur[0], rcur[1] = rn, rnB
                    nd, ndB = ndr.next()
                    S.op("dve", lambda: nc.vector.scalar_tensor_tensor(out=nd[:, 0:129], in0=psx[:, 0:129], scalar=col(2), in1=t1[:],
                                                                       op0=ALU.mult, op1=ALU.add),
                         reads=[psxB, t1B, colsB], writes=[ndB])
                    S.op("dve", lambda: nc.vector.scalar_tensor_tensor(out=nd[:, 129:130], in0=nd[:, 128:129], scalar=-1.0,
                                                                       in1=nd[:, 128:129], op0=ALU.mult, op1=ALU.max),
                         reads=[ndB], writes=[ndB])
                    S.op("dve", lambda: nc.vector.tensor_tensor(out=nd[:, 130:131], in0=nd[:, 129:130], in1=col(3), op=ALU.max),
                         reads=[ndB, colsB], writes=[ndB])
                    S.op("dve", lambda: nc.vector.reciprocal(out=nd[:, 131:132], in_=nd[:, 130:131]), reads=[ndB], writes=[ndB])
                    S.op("dve", lambda: nc.vector.tensor_scalar(out=nd[:, 0:128], in0=nd[:, 0:128], scalar1=nd[:, 131:132],
                                                                scalar2=None, op0=ALU.mult), reads=[ndB], writes=[ndB])
                    groupnorm_gate(nd[:, 0:128], ndB, AF.Sigmoid, 2 + hh, c)
                hA = ml_A(0)
                for c in range(NCH):
                    hN = ml_A(c + 1) if c + 1 < NCH else None
                    ml_C(c, hA)
                    hA = hN
                P["ystore"](S, 256 + hh * 128, Yst, YB)
            S.barrier()

        with cx.stack() as es:
            QT = cx.sb(es, [128, SEQ], BF16, "fQT")
            KT = cx.sb(es, [128, SEQ], BF16, "fKT")
            V1 = cx.sb(es, [128, NCH, 129], BF16, "fV1")
            Yst = cx.sb(es, [128, NCH, 128], BF16, "fYst")
            mskd = cx.sb(es, [128, 128], F32, "mskd")
            kB = Buf("fconst")
            S.dma("sp", mskd[:], mask_d, writes=[kB])
            QB, KB_, VB, YB = Buf("fQT"), Buf("fKT"), Buf("fV1"), Buf("fYst")
            S.op("dve", lambda: nc.vector.memset(V1[:, :, 128:129], 1.0), writes=[VB])
            ps_s = cx.psring(es, 4, [128, 128], F32, "fps_s")
            ps_o = cx.psring(es, 2, [128, 256], F32, "fps_o")
            PTr = cx.sbring(es, 4, [128, 128], BF16, "fPT")
            rdr = cx.sbring(es, 2, [128, 2], F32, "frd")
            for h in range(4):
                S.dma("sp", QT[:], FMs[h], reads=[FMsB], writes=[QB])
                S.dma("sp", KT[:], FMs[4 + h], reads=[FMsB], writes=[KB_])
                S.dma("sp", V1[:, :, 0:128], TMs[:, 1024 + h * 128:1024 + (h + 1) * 128].rearrange("(c p) e -> p c e", p=128),
                      reads=[TMsB], writes=[VB])
                for qb in range(NCH):
                    qsl = slice(qb * 128, (qb + 1) * 128)
                    pso, psoB = ps_o.next()
                    pend = {}
                    LOOK = 2

                    def issue_s(kb, qsl=qsl, pend=pend):
                        ksl = slice(kb * 128, (kb + 1) * 128)
                        pss, pssB = ps_s.next()
                        S.op("pe", lambda: nc.tensor.matmul(pss[:], KT[:, ksl], QT[:, qsl], start=True, stop=True),
                             reads=[KB_, QB], writes=[pssB])
                        pend[kb] = (pss, pssB)
                    for kb in range(min(LOOK, qb + 1)):
                        issue_s(kb)
                    for kb in range(qb + 1):
                        if kb + LOOK <= qb:
                            issue_s(kb + LOOK)
                        pss, pssB = pend.pop(kb)
                        PT, PTB = PTr.next()
                        S.op("act", lambda: nc.scalar.activation(
                            out=PT[:], in_=pss[:], func=AF.Exp, scale=ISQ, bias=tab[:, h, qb, kb:kb + 1]),
                             reads=[pssB, tabB], writes=[PTB])
                        if kb == qb:
                            S.op("dve", lambda: nc.vector.tensor_tensor(out=PT[:], in0=PT[:], in1=mskd[:], op=ALU.mult),
                                 reads=[PTB, kB], writes=[PTB])
                        S.op("pe", lambda: nc.tensor.matmul(pso[:, 0:129], PT[:], V1[:, kb, :],
                                                            start=(kb == 0), stop=(kb == qb)),
                             reads=[PTB, VB], writes=[psoB])
                    rd, rdB = rdr.next()
                    S.op("dve", lambda rd=rd, pso=pso: nc.vector.reciprocal(out=rd[:, 0:1], in_=pso[:, 128:129]),
                         reads=[psoB], writes=[rdB])
                    S.op("dve", lambda rd=rd, pso=pso, qb=qb: nc.vector.tensor_scalar(
                        out=Yst[:, qb, :], in0=pso[:, 0:128], scalar1=rd[:, 0:1], scalar2=None, op0=ALU.mult),
                         reads=[psoB, rdB], writes=[YB])
                P["ystore"](S, 512 + h * 128, Yst, YB)
            S.barrier()


def _c_decl(nc, sfx=""):
    def din(name, shape, dt=F32):
        return nc.dram_tensor(name + sfx, list(shape), dt, kind="ExternalInput").ap()
    P = {}
    P["adaw_b"] = din("adaw_b", [D, 2 * D])
    P["adaw_c"] = din("adaw_c", [D, 2 * D])
    P["adab_rep"] = din("adab_rep", [128, 2 * D])
    P["adab_col2"] = din("adab_col2", [128, 32])
    P["g2col"] = din("g2col", [128, KC])
    P["w_out"] = din("w_out", [D, D])
    P["w_r"] = din("w_r", [D, 36])
    P["b_r"] = din("b_r", [128, 36])
    P["w1"] = din("w1", [NEXP, D, DEXP])
    P["w3"] = din("w3", [NEXP, D, DEXP])
    P["w2"] = din("w2", [NEXP, DEXP, D])
    return P


def _c_scratch(nc):
    P = {}
    P["x1s"] = nc.dram_tensor("x1s", [TOKC, D], F32, kind="Internal").ap()
    P["h2s"] = nc.dram_tensor("h2s", [KC, 128, TOKC], BF16, kind="Internal").ap()
    P["x1B"], P["h2B"] = Buf("x1s"), Buf("h2s")
    return P


def build_c(last):
    nc = bass.Bass("TRN2", target_bir_lowering=False)
    cx = Ctx(nc)
    S = cx.S

    def din(name, shape, dt=F32):
        return nc.dram_tensor(name, list(shape), dt, kind="ExternalInput").ap()
    P = {}
    P["c_rep"] = din("c_rep", [128, KC, 128])
    P["ident_f"] = din("ident_f", [128, 128])
    P["fg_rep"] = din("fg_rep", [128, D])
    P.update(_c_decl(nc))
    P.update(_c_scratch(nc))
    x = din("x", [TOKC, D])
    yA = din("yA", [TOKC, 1024], BF16)
    yB_ = din("yB", [TOKC, 1024], BF16)
    out = nc.dram_tensor("out", [TOKC, D], F32, kind="ExternalOutput").ap()
    outB = Buf("out")
    P["xsrc"] = lambda t0: x[t0:t0 + 128, :]
    P["xreads"] = []

    def yload(S, tt, yc, ycB, ycB2):
        t0 = tt * 128
        S.dma("sp", yc[:, 0:1024], yA[t0:t0 + 128, :], writes=[ycB])
        S.dma("sp", yc[:, 1024:2048], yB_[t0:t0 + 128, :], reads=[ycB], writes=[ycB2])
    P["yload"] = yload

    def ostore(S, t0, xt, xB):
        S.dma("sp", out[t0:t0 + 128, :], xt[:], reads=[xB], writes=[outB])
    P["ostore"] = ostore
    emit_c(cx, P, last)
    S.close()
    return nc


def emit_c(cx, P, last):
    nc = cx.nc
    S = cx.S
    c_rep, adaw_b, adaw_c, adab_rep, adab_col, g2col = (P["c_rep"], P["adaw_b"], P["adaw_c"], P["adab_rep"],
                                                       P["adab_col2"], P["g2col"])
    w_out, w_r, b_r, w1, w3, w2, ident_f, fg_rep = (P["w_out"], P["w_r"], P["b_r"], P["w1"], P["w3"], P["w2"],
                                                    P["ident_f"], P["fg_rep"])
    x1s, h2s, x1B, h2B = P["x1s"], P["h2s"], P["x1B"], P["h2B"]

    with cx.stack() as es0:
        identf = cx.sb(es0, [128, 128], F32, "identf")
        identb = cx.sb(es0, [128, 128], BF16, "identb")
        epsc = cx.sb(es0, [128, 1], F32, "epsc")
        cB = Buf("consts")
        S.dma("sp", identf[:], ident_f, writes=[cB])
        S.op("dve", lambda: nc.vector.tensor_copy(out=identb[:], in_=identf[:]), reads=[cB], writes=[cB])
        S.op("dve", lambda: nc.vector.memset(epsc[:], EPS), writes=[cB])
        g2t = cx.sb(es0, [128, D], F32, "g2t")
        gBB = Buf("gBt")
        comb = cx.sb(es0, [128, TOKC // 128, NEXP], F32, "comb")
        combB = Buf("comb")

        with cx.stack() as es:
            scale_col = cx.sb(es, [128, KC], F32, "scalecol")
            shift_col = cx.sb(es, [128, KC], F32, "shiftcol")
            modB = Buf("mod")
            cact = cx.sb(es, [128, KC, 128], BF16, "cact")
            cactB = Buf("cact")
            g1t = cx.sb(es, [128, D], F32, "g1t")
            wring = cx.sbring(es, 2, [128, KC, 512], BF16, "wp")
            mmring = cx.psring(es, 2, [128, 512], F32, "mm")
            with cx.stack() as est:
                ctmp = cx.sb(est, [128, KC, 128], F32, "ctmp")
                emit_cact(cx, est, c_rep, cact, cactB, ctmp)
                modps = cx.ps(est, [128, 32], F32, "modps")
                modpsB = Buf("modps")
                modcol = cx.sb(est, [128, 32], F32, "modcol")
                adabc = cx.sb(est, [128, 32], F32, "adabc")
                g2c = cx.sb(est, [128, KC], F32, "g2c")
                aB = Buf("adab")
                S.dma("sp", adabc[:], adab_col, writes=[aB])
                S.dma("sp", g2c[:], g2col, writes=[aB])
                emit_mod_cols(cx, est, adaw_c, cact, cactB, adabc, modcol, modB, 32, wring, modps, modpsB)
                S.op("dve", lambda: nc.vector.tensor_copy(out=shift_col[:], in_=modcol[:, 0:KC]), reads=[modB], writes=[modB])
                S.op("dve", lambda: nc.vector.scalar_tensor_tensor(out=scale_col[:], in0=modcol[:, KC:2 * KC], scalar=1.0,
                                                                   in1=g2c[:], op0=ALU.add, op1=ALU.mult),
                     reads=[modB, aB], writes=[modB])
                abr = cx.sb(est, [128, 2 * D], F32, "abr")
                S.dma("sp", abr[:], adab_rep, writes=[aB])
                for pc in range(8):
                    wt, wb = wring.next()
                    S.dma("pool", wt[:], adaw_b[:, pc * 512:(pc + 1) * 512].rearrange("(k p) n -> p k n", p=128), writes=[wb])
                    mm, mmB = mmring.next()
                    S.group("pe", [lambda k=k, mm=mm, wt=wt: nc.tensor.matmul(mm[:], cact[:, k, :], wt[:, k, :],
                                                                           start=(k == 0), stop=(k == KC - 1))
                                   for k in range(KC)], reads=[wb, cactB], writes=[mmB])
                    S.op("dve", lambda pc=pc, mm=mm: nc.vector.tensor_tensor(
                        out=(g1t if pc < 4 else g2t)[:, (pc % 4) * 512:(pc % 4 + 1) * 512], in0=mm[:],
                        in1=abr[:, pc * 512:(pc + 1) * 512], op=ALU.add), reads=[mmB, aB], writes=[gBB])
                S.barrier()

            wo = cx.sb(es, [128, KC, D], BF16, "wo")
            woB = Buf("wo")
            for k4 in range(4):
                S.dma("pool", wo[:, k4 * 4:(k4 + 1) * 4, :],
                      w_out[k4 * 512:(k4 + 1) * 512, :].rearrange("(k p) n -> p k n", p=128), writes=[woB])
            wr = cx.sb(es, [128, KC, 36], F32, "wr")
            br = cx.sb(es, [128, 36], F32, "br")
            wrB = Buf("wr")
            S.dma("sp", wr[:], w_r.rearrange("(k p) n -> p k n", p=128), writes=[wrB])
            S.dma("sp", br[:], b_r, writes=[wrB])
            ycr = cx.sbring(es, 2, [128, D], BF16, "yc")
            if "c1_hook" in P:
                P["c1_hook"](cx, es)
            yTr = cx.sbring(es, 2, [128, KC, 128], BF16, "yT")
            xring = cx.sbring(es, 2, [128, D], F32, "xt")
            xnring = cx.sbring(es, 1, [128, D], F32, "xn")
            junk = cx.sb(es, [128, D], BF16, "junk")
            junkB = Buf("junk")
            ssring = cx.sbring(es, 2, [128, 4], F32, "ss")
            tpb = cx.psring(es, 1, [128, KC, 128], BF16, "tpb")
            tpf = cx.psring(es, 2, [128, 4, 128], F32, "tpf")
            h2fr = cx.sbring(es, 1, [128, KC, 128], F32, "h2f")
            h2br = cx.sbring(es, 2, [128, KC, 128], BF16, "h2b")
            tmr = cx.sbring(es, 2, [128, 512], F32, "tm")
            psr = cx.psring(es, 1, [128, 64], F32, "psr")
            rt = cx.sbring(es, 2, [128, 96], F32, "rt")
            for tt in range(TOKC // 128):
                t0 = tt * 128
                yc, ycB = ycr.next()
                ycB2 = Buf("yc2")
                P["yload"](S, tt, yc, ycB, ycB2)
                tp, tpB = tpb.next()
                S.group("pe", [lambda k=k: nc.tensor.transpose(tp[:, k, :], yc[:, k * 128:(k + 1) * 128], identb[:])
                               for k in range(KC)], reads=[ycB, ycB2, cB], writes=[tpB])
                yT, yTB = yTr.next()
                S.op("act", lambda: nc.scalar.copy(out=yT[:, 0:8, :], in_=tp[:, 0:8, :]), reads=[tpB], writes=[yTB])
                yTB2 = Buf("yT2")
                S.op("dve", lambda: nc.vector.tensor_copy(out=yT[:, 8:16, :], in_=tp[:, 8:16, :]), reads=[tpB, yTB], writes=[yTB2])
                xt, xB = xring.next()
                S.dma("sp", xt[:], P["xsrc"](t0), reads=P["xreads"], writes=[xB])
                for cb in range(4):
                    csl = slice(cb * 512, (cb + 1) * 512)
                    mm, mmB = mmring.next()
                    S.group("pe", [lambda k=k, mm=mm: nc.tensor.matmul(mm[:], yT[:, k, :], wo[:, k, csl],
                                                                        start=(k == 0), stop=(k == KC - 1))
                                   for k in range(KC)], reads=[yTB, yTB2, woB], writes=[mmB])
                    tm, tmB = tmr.next()
                    S.op("dve", lambda: nc.vector.tensor_tensor(out=tm[:], in0=mm[:], in1=g1t[:, csl], op=ALU.mult),
                         reads=[mmB, gBB], writes=[tmB])
                    S.op("dve", lambda: nc.vector.tensor_tensor(out=xt[:, csl], in0=xt[:, csl], in1=tm[:], op=ALU.add),
                         reads=[tmB, xB], writes=[xB])
                S.dma("sp", x1s[t0:t0 + 128, :], xt[:], reads=[xB], writes=[x1B])
                ss, ssB = ssring.next()
                emit_rms_stats(cx, xt, xB, junk, junkB, ss, ssB, epsc)
                xn, xnB = xnring.next()
                S.op("dve", lambda: nc.vector.tensor_scalar(out=xn[:], in0=xt[:], scalar1=ss[:, 2:3], scalar2=None,
                                                            op0=ALU.mult), reads=[xB, ssB], writes=[xnB])
                h2f, h2fB = h2fr.next()
                for k4 in range(4):
                    tf, tfB = tpf.next()
                    S.group("pe", [lambda j=j, tf=tf, k4=k4: nc.tensor.transpose(
                        tf[:, j, :], xn[:, (k4 * 4 + j) * 128:(k4 * 4 + j + 1) * 128], identf[:]) for j in range(4)],
                            reads=[xnB, cB], writes=[tfB])
                    for j in range(4):
                        k = k4 * 4 + j
                        S.op("act", lambda j=j, k=k, tf=tf: nc.scalar.activation(
                            out=h2f[:, k, :], in_=tf[:, j, :], func=AF.Identity, scale=scale_col[:, k:k + 1],
                            bias=shift_col[:, k:k + 1]), reads=[tfB, modB], writes=[h2fB])
                h2b, h2bB = h2br.next()
                S.op("dve", lambda: nc.vector.tensor_copy(out=h2b[:], in_=h2f[:]), reads=[h2fB], writes=[h2bB])
                S.dma("sp", h2s[:, :, t0:t0 + 128].rearrange("k p t -> p k t"), h2b[:], reads=[h2bB], writes=[h2B])
                pr, prB = psr.next()
                S.group("pe", [lambda k=k: nc.tensor.matmul(pr[:, 0:36], h2f[:, k, :], wr[:, k, :],
                                                            start=(k == 0), stop=(k == KC - 1)) for k in range(KC)],
                        reads=[h2fB, wrB], writes=[prB])
                r, rB_ = rt.next()
                V = nc.vector

                def dv(fn):
                    S.op("dve", fn, reads=[rB_], writes=[rB_])
                S.op("dve", lambda: V.tensor_tensor(out=r[:, 0:36], in0=pr[:, 0:36], in1=br[:], op=ALU.add),
                     reads=[prB, wrB], writes=[rB_])
                dv(lambda: V.reduce_max(out=r[:, 40:41], in_=r[:, 0:4], axis=AX.X))
                dv(lambda: V.tensor_scalar(out=r[:, 36:40], in0=r[:, 0:4], scalar1=r[:, 40:41], scalar2=None, op0=ALU.is_equal))
                dv(lambda: V.tensor_scalar(out=r[:, 88:92], in0=r[:, 0:4], scalar1=r[:, 40:41], scalar2=None, op0=ALU.subtract))
                S.op("act", lambda: nc.scalar.activation(out=r[:, 88:92], in_=r[:, 88:92], func=AF.Exp, accum_out=r[:, 41:42]),
                     reads=[rB_], writes=[rB_])
                dv(lambda: V.reciprocal(out=r[:, 42:43], in_=r[:, 41:42]))
                dv(lambda: V.tensor_scalar(out=r[:, 44:52], in0=r[:, 4:12], scalar1=r[:, 36:37], scalar2=None, op0=ALU.mult))
                for gg in range(1, 4):
                    dv(lambda gg=gg: V.scalar_tensor_tensor(out=r[:, 44:52], in0=r[:, 4 + gg * 8:12 + gg * 8],
                                                            scalar=r[:, 36 + gg:37 + gg], in1=r[:, 44:52],
                                                            op0=ALU.mult, op1=ALU.add))
                dv(lambda: V.reduce_max(out=r[:, 76:77], in_=r[:, 44:52], axis=AX.X))
                dv(lambda: V.tensor_scalar(out=r[:, 52:60], in0=r[:, 44:52], scalar1=r[:, 76:77], scalar2=None, op0=ALU.is_equal))
                dv(lambda: V.scalar_tensor_tensor(out=r[:, 60:68], in0=r[:, 52:60], scalar=NEG, in1=r[:, 44:52],
                                                  op0=ALU.mult, op1=ALU.add))
                dv(lambda: V.reduce_max(out=r[:, 77:78], in_=r[:, 60:68], axis=AX.X))
                dv(lambda: V.tensor_scalar(out=r[:, 68:76], in0=r[:, 60:68], scalar1=r[:, 77:78], scalar2=None, op0=ALU.is_equal))
                dv(lambda: V.tensor_tensor(out=r[:, 78:79], in0=r[:, 76:77], in1=r[:, 77:78], op=ALU.subtract))
                S.op("act", lambda: nc.scalar.activation(out=r[:, 78:79], in_=r[:, 78:79], func=AF.Sigmoid),
                     reads=[rB_], writes=[rB_])
                dv(lambda: V.tensor_tensor(out=r[:, 78:79], in0=r[:, 78:79], in1=r[:, 42:43], op=ALU.mult))
                dv(lambda: V.tensor_tensor(out=r[:, 79:80], in0=r[:, 42:43], in1=r[:, 78:79], op=ALU.subtract))
                dv(lambda: V.tensor_scalar(out=r[:, 80:88], in0=r[:, 52:60], scalar1=r[:, 78:79], scalar2=None, op0=ALU.mult))
                dv(lambda: V.scalar_tensor_tensor(out=r[:, 80:88], in0=r[:, 68:76], scalar=r[:, 79:80], in1=r[:, 80:88],
                                                  op0=ALU.mult, op1=ALU.add))
                for gg in range(4):
                    S.op("dve", lambda gg=gg: V.tensor_scalar(out=comb[:, tt, gg * 8:(gg + 1) * 8], in0=r[:, 80:88],
                                                              scalar1=r[:, 36 + gg:37 + gg], scalar2=None, op0=ALU.mult),
                         reads=[rB_], writes=[combB])
            S.barrier()

        with cx.stack() as es:
            TB = 1024
            acc = cx.sb(es, [128, TB // 128, D], F32, "acc")
            accB = [Buf("acc%d" % i) for i in range(TB // 128)]
            hT = cx.sb(es, [128, KC, TB], BF16, "hT")
            hB = Buf("hT")
            wring = cx.sbring(es, 4, [128, 8192], BF16, "wexp")
            G = cx.sbring(es, 2, [128, 4, 512], BF16, "G")
            sil = cx.sbring(es, 2, [128, 512], F32, "sil")
            ps13 = cx.psring(es, 4, [128, 512], F32, "ps13")
            psy = cx.psring(es, 4, [128, 512], F32, "psy")
            xring = cx.sbring(es, 1, [128, D], F32, "xt2")
            fgr = None
            if last:
                fgr = cx.sb(es, [128, D], F32, "fgr")
                fgB = Buf("fgr")
                S.dma("sp", fgr[:], fg_rep, writes=[fgB])
                junk = cx.sb(es, [128, D], BF16, "junk2")
                junkB = Buf("junk2")
                ssring = cx.sbring(es, 2, [128, 4], F32, "ss2")
            for ps_ in range(TOKC // TB):
                tok0 = ps_ * TB
                for k4 in range(4):
                    S.dma("sp", hT[:, k4 * 4:(k4 + 1) * 4, :],
                          h2s[k4 * 4:(k4 + 1) * 4, :, tok0:tok0 + TB].rearrange("k p t -> p k t"),
                          reads=[h2B], writes=[hB] if k4 == 0 else [Buf("hTx%d" % k4)])
                S.barrier(["pe"])
                for e in range(NEXP):
                    w1t, w1B = wring.next()
                    S.dma("pool", w1t[:].rearrange("p (k n) -> p k n", n=DEXP), w1[e].rearrange("(k p) n -> p k n", p=128),
                          writes=[w1B])
                    w3t, w3B = wring.next()
                    S.dma("pool", w3t[:].rearrange("p (k n) -> p k n", n=DEXP), w3[e].rearrange("(k p) n -> p k n", p=128),
                          writes=[w3B])
                    w2t, w2B = wring.next()
                    S.dma("pool", w2t[:].rearrange("p (k n) -> p k n", n=D), w2[e].rearrange("(k p) n -> p k n", p=128),
                          writes=[w2B])
                    w1v = w1t[:].rearrange("p (k n) -> p k n", n=DEXP)
                    w3v = w3t[:].rearrange("p (k n) -> p k n", n=DEXP)
                    w2v = w2t[:].rearrange("p (k n) -> p k n", n=D)
                    for half in range(TB // 512):
                        tsl = slice(half * 512, (half + 1) * 512)
                        Gt, GB = G.next()
                        for hc in range(4):
                            hsl = slice(hc * 128, (hc + 1) * 128)
                            p1, p1B = ps13.next()
                            S.group("pe", [lambda k=k, p1=p1: nc.tensor.matmul(p1[:], w1v[:, k, hsl], hT[:, k, tsl],
                                                                                start=(k == 0), stop=(k == KC - 1))
                                           for k in range(KC)], reads=[w1B, hB], writes=[p1B])
                            p3, p3B = ps13.next()
                            S.group("pe", [lambda k=k, p3=p3: nc.tensor.matmul(p3[:], w3v[:, k, hsl], hT[:, k, tsl],
                                                                                start=(k == 0), stop=(k == KC - 1))
                                           for k in range(KC)], reads=[w3B, hB], writes=[p3B])
                            sl_, slB = sil.next()
                            S.op("act", lambda: nc.scalar.activation(out=sl_[:], in_=p1[:], func=AF.Silu), reads=[p1B], writes=[slB])
                            S.op("dve", lambda: nc.vector.tensor_tensor(out=Gt[:, hc, :], in0=p3[:], in1=sl_[:], op=ALU.mult),
                                 reads=[p3B, slB], writes=[GB])
                        for t4 in range(4):
                            tt = half * 4 + t4
                            gt = ps_ * (TB // 128) + tt
                            for cb in range(4):
                                csl = slice(cb * 512, (cb + 1) * 512)
                                py, pyB = psy.next()
                                S.group("pe", [lambda hc=hc, py=py: nc.tensor.matmul(
                                    py[:], Gt[:, hc, t4 * 128:(t4 + 1) * 128], w2v[:, hc, csl],
                                    start=(hc == 0), stop=(hc == 3)) for hc in range(4)], reads=[GB, w2B], writes=[pyB])
                                if e == 0:
                                    S.op("dve", lambda: nc.vector.tensor_scalar(out=acc[:, tt, csl], in0=py[:],
                                                                                scalar1=comb[:, gt, e:e + 1], scalar2=None,
                                                                                op0=ALU.mult),
                                         reads=[pyB, combB], writes=[accB[tt]])
                                else:
                                    S.op("dve", lambda: nc.vector.scalar_tensor_tensor(
                                        out=acc[:, tt, csl], in0=py[:], scalar=comb[:, gt, e:e + 1], in1=acc[:, tt, csl],
                                        op0=ALU.mult, op1=ALU.add), reads=[pyB, combB, accB[tt]], writes=[accB[tt]])
                for tt in range(TB // 128):
                    t0 = tok0 + tt * 128
                    xt, xB = xring.next()
                    S.dma("sp", xt[:], x1s[t0:t0 + 128, :], reads=[x1B], writes=[xB])
                    S.op("dve", lambda: nc.vector.tensor_tensor(out=acc[:, tt, :], in0=acc[:, tt, :], in1=g2t[:], op=ALU.mult),
                         reads=[accB[tt], gBB], writes=[accB[tt]])
                    S.op("dve", lambda: nc.vector.tensor_tensor(out=xt[:], in0=xt[:], in1=acc[:, tt, :], op=ALU.add),
                         reads=[accB[tt], xB], writes=[xB])
                    if last:
                        ss, ssB = ssring.next()
                        emit_rms_stats(cx, xt, xB, junk, junkB, ss, ssB, epsc)
                        S.op("dve", lambda: nc.vector.scalar_tensor_tensor(out=xt[:], in0=xt[:], scalar=ss[:, 2:3], in1=fgr[:],
                                                                           op0=ALU.mult, op1=ALU.mult),
                             reads=[xB, ssB, fgB], writes=[xB])
                    P["ostore"](S, t0, xt, xB)
                S.barrier()


OFF = dict(rq=0, rk=512, rv=1024, rg=1536, mx=2048, mv=2560, mo=3072, mi=3584, mf=3588,
           fq=3592, fk=4616, fv=5640, ff=6664)
_CONST = {}
_PROG = {}


def _consts():
    if _CONST:
        return _CONST
    half = 64
    inv = (10000.0 ** (-np.arange(half, dtype=np.float32) / np.float32(half))).astype(np.float32)
    pos = np.arange(SEQ, dtype=np.float32)
    ang = (pos[:, None] * inv[None, :]).astype(np.float32)
    cos = np.cos(ang).astype(np.float32).T
    sin = np.sin(ang).astype(np.float32).T
    _CONST["rotC"] = np.ascontiguousarray(np.concatenate([cos, cos], 0))
    _CONST["rotS"] = np.ascontiguousarray(np.concatenate([-sin, sin], 0))
    _CONST["ident_f"] = np.eye(128, dtype=np.float32)
    j = np.arange(128)[:, None]
    i = np.arange(128)[None, :]
    _CONST["mask_m"] = np.where(j <= i, ISQ, 0.0).astype(np.float32)
    _CONST["mask_d"] = np.where(j <= i, 1.0, 0.0).astype(np.float32)
    rows = np.zeros((4, 3, SEQ), np.float32)
    rows[:, 0, :] = 1.0
    rows[:, 1, :] = 1.0
    rows[:, 1, ::128] = 0.0
    rows[:, 2, ::128] = NEG
    _CONST["rows_c"] = rows
    oh = np.zeros((4, 4, 128), np.float32)
    for h in range(4):
        oh[h, h, :] = 1.0
    _CONST["onehot"] = oh
    DT = np.zeros((4, 128, 128), np.float64)
    cols = np.zeros((4, 128, 3), np.float64)
    for h in range(4):
        lg = np.log1p(-(2.0 ** (-5.0 - h)))
        diff = (i - j).astype(np.float64)
        DT[h] = np.where(diff >= 0, np.exp(np.maximum(diff, 0) * lg), 0.0) * ISQ
        p = np.arange(128, dtype=np.float64)
        cols[h, :, 0] = np.exp((127 - p) * lg) * ISQ
        cols[h, :, 1] = np.exp((p + 1) * lg)
        cols[h, :, 2] = np.exp(128 * lg)
    _CONST["DT"] = DT.astype(np.float32)
    _CONST["rcols"] = cols.astype(np.float32)
    return _CONST


def _col_layout(v, n):
    return np.ascontiguousarray(v.reshape(n, 128).T)


def _prog(kind):
    if kind not in _PROG:
        _PROG[kind] = build_ab() if kind == "ab" else build_c(kind == "c_last")
    return _PROG[kind]


def _ab_inputs(l, b, hh, xb, inp):
    K = _consts()
    w_in = inp["w_in"][l]
    perm = np.concatenate([np.arange(64, 128), np.arange(0, 64)])
    cols = []
    for i in range(4):
        cols.append(np.arange(128) + OFF["fq"] + (hh * 4 + i) * 128)
    for i in range(4):
        cols.append(np.arange(128) + OFF["fk"] + (hh * 4 + i) * 128)
    for i in range(2):
        cols.append(np.arange(128) + OFF["rq"] + (hh * 2 + i) * 128)
    for i in range(2):
        cols.append(np.arange(128) + OFF["rk"] + (hh * 2 + i) * 128)
    for i in range(2):
        cols.append(perm + OFF["rq"] + (hh * 2 + i) * 128)
    for i in range(2):
        cols.append(perm + OFF["rk"] + (hh * 2 + i) * 128)
    for i in range(2):
        cols.append(np.arange(128) + OFF["mx"] + (hh * 2 + i) * 128)
    fm_cols = np.concatenate(cols)
    tm_cols = np.concatenate([np.arange(256) + OFF["rv"] + hh * 256, np.arange(256) + OFF["rg"] + hh * 256,
                              np.arange(256) + OFF["mv"] + hh * 256, np.arange(256) + OFF["mo"] + hh * 256,
                              np.arange(512) + OFF["fv"] + hh * 512])
    g_cols = np.concatenate([np.arange(2) + OFF["mi"] + hh * 2, np.arange(2) + OFF["mf"] + hh * 2,
                             np.arange(4) + OFF["ff"] + hh * 4])
    c = inp["c"][b]
    gate_b = np.zeros((4, 3), np.float32)
    gate_b[0:2, 0] = inp["mlstm_i_b"][l][hh * 2:hh * 2 + 2]
    gate_b[0:2, 1] = inp["mlstm_f_b"][l][hh * 2:hh * 2 + 2]
    gate_b[:, 2] = inp["fox_f_b"][l][hh * 4:hh * 4 + 4]
    gn = np.concatenate([inp["ret_gn_g"][l][hh * 256:(hh + 1) * 256], inp["mlstm_gn_g"][l][hh * 256:(hh + 1) * 256]])
    conv = np.zeros((128, 2, 5), np.float32)
    for i in range(2):
        sl = slice((hh * 2 + i) * 128, (hh * 2 + i + 1) * 128)
        conv[:, i, 0:4] = inp["mlstm_conv_w"][l][:, sl].T
        conv[:, i, 4] = inp["mlstm_conv_b"][l][sl]
    return {
        "x": xb,
        "c_rep": np.ascontiguousarray(np.broadcast_to(_col_layout(c, KC)[:, :, None], (128, KC, 128))),
        "adaw": np.ascontiguousarray(inp["ada_w"][l][:, 0:2 * D]),
        "adab_col": _col_layout(inp["ada_b"][l][0:2 * D], 32),
        "g1col": _col_layout(inp["norm1_g"][l], KC),
        "w_fm": np.ascontiguousarray(w_in[:, fm_cols]),
        "w_tm": np.ascontiguousarray(w_in[:, tm_cols]),
        "w_g": np.ascontiguousarray(w_in[:, g_cols]),
        "ident_f": K["ident_f"], "rotC": K["rotC"], "rotS": K["rotS"],
        "ret_DT": np.ascontiguousarray(K["DT"][hh * 2:hh * 2 + 2].transpose(1, 0, 2)),
        "ret_cols": np.ascontiguousarray(K["rcols"][hh * 2:hh * 2 + 2].transpose(1, 0, 2)),
        "gn_rep": np.ascontiguousarray(np.broadcast_to(gn.reshape(1, 4, 128), (128, 4, 128))),
        "conv_cols": conv,
        "wq": np.ascontiguousarray(inp["mlstm_wq"][l][hh * 2:hh * 2 + 2]),
        "wk": np.ascontiguousarray(inp["mlstm_wk"][l][hh * 2:hh * 2 + 2]),
        "gate_b": gate_b, "rows_c": K["rows_c"], "mask_m": K["mask_m"], "mask_d": K["mask_d"], "onehot": K["onehot"],
    }


def _c_inputs(l, b, th, xb, ys, inp, shared):
    K = _consts()
    rows = slice(th * TOKC, (th + 1) * TOKC)
    c = inp["c"][b]
    d = {
        "x": np.ascontiguousarray(xb[rows]),
        "yA": np.ascontiguousarray(ys[0][rows]),
        "yB": np.ascontiguousarray(ys[1][rows]),
        "c_rep": np.ascontiguousarray(np.broadcast_to(_col_layout(c, KC)[:, :, None], (128, KC, 128))),
        "ident_f": K["ident_f"],
    }
    d.update(shared)
    return d


def _c_shared(l, inp):
    ada_w = inp["ada_w"][l]
    ada_b = inp["ada_b"][l]
    gsel = np.concatenate([np.arange(2 * D, 3 * D), np.arange(5 * D, 6 * D)])
    wo = inp["w_out"][l]
    ro = np.concatenate([np.arange(0, 256), np.arange(512, 768), np.arange(1024, 1536),
                         np.arange(256, 512), np.arange(768, 1024), np.arange(1536, 2048)])
    br = np.concatenate([inp["router_group_b"][l], inp["router_expert_b"][l]])
    return {
        "adaw_b": np.ascontiguousarray(ada_w[:, gsel]),
        "adaw_c": np.ascontiguousarray(ada_w[:, 3 * D:5 * D]),
        "adab_rep": np.ascontiguousarray(np.broadcast_to(ada_b[gsel][None, :], (128, 2 * D))),
        "adab_col2": _col_layout(ada_b[3 * D:5 * D], 32),
        "g2col": _col_layout(inp["norm2_g"][l], KC),
        "w_out": np.ascontiguousarray(wo[ro, :]),
        "w_r": np.ascontiguousarray(np.concatenate([inp["router_group_w"][l], inp["router_expert_w"][l]], axis=1)),
        "b_r": np.ascontiguousarray(np.broadcast_to(br[None, :], (128, 36))),
        "w1": inp["moe_w1"][l], "w3": inp["moe_w3"][l], "w2": inp["moe_w2"][l],
        "fg_rep": np.ascontiguousarray(np.broadcast_to(inp["final_g"][None, :], (128, D))),
    }


AB_LAYER_KEYS = ("adaw", "adab_col", "g1col", "w_fm", "w_tm", "w_g", "gn_rep", "conv_cols", "wq", "wk", "gate_b")
C_LAYER_KEYS = ("adaw_b", "adaw_c", "adab_rep", "adab_col2", "g2col", "w_out", "w_r", "b_r", "w1", "w3", "w2")
AB_CONST_KEYS = ("c_rep", "ident_f", "rotC", "rotS", "ret_DT", "ret_cols", "rows_c", "mask_m", "mask_d", "onehot")
GROUPS = [[0, 1], [2, 3], [4, 5], [6, 7]]
NYC = 4
NXC = 8


def build_fused(depth=DEPTH):
    nc = bass.Bass("TRN2", target_bir_lowering=False)
    cx = Ctx(nc)
    S = cx.S

    def din(name, shape, dt=F32):
        return nc.dram_tensor(name, list(shape), dt, kind="ExternalInput").ap()

    def dint(name, shape, dt):
        return nc.dram_tensor(name, list(shape), dt, kind="Internal").ap()
    consts = _ab_const_decl(nc)
    fg_rep = din("fg_rep", [128, D])
    msel_d = din("msel", [128, 2])
    x_all0 = din("x_all0", [SEQ, D])
    x_half0 = din("x_half0", [TOKC, D])
    out = nc.dram_tensor("out", [TOKC, D], F32, kind="ExternalOutput").ap()
    outB = Buf("out")
    abs_ = _ab_scratch(nc)
    cs_ = _c_scratch(nc)
    ysend = [dint("ysend%d" % c, [SEQ // NYC, 1024], BF16) for c in range(NYC)]
    yall = [dint("yall%d" % c, [2 * SEQ // NYC, 1024], BF16) for c in range(NYC)]
    xloc = [dint("xloc%d" % c, [TOKC // NXC, D], F32) for c in range(NXC)]
    xall = [dint("xall%d" % c, [2 * TOKC // NXC, D], F32) for c in range(NXC)]
    ysB, yaB, xlB, xaB = Buf("ysend"), Buf("yall"), Buf("xloc"), Buf("xall")
    YR = SEQ // NYC
    XR = TOKC // NXC
    ccsems = []

    def gather(srcs, dsts):
        S.barrier()
        S.new_epoch()
        toks = []
        for s_, d_ in zip(srcs, dsts):
            g = nc.semaphore("cc%d" % len(ccsems))
            sem = g.__enter__()
            ccsems.append(g)
            nc.gpsimd.collective_compute("AllGather", ALU.bypass, replica_groups=GROUPS, ins=[s_], outs=[d_]).then_inc(sem)
            toks.append((sem, 1))
        for e in S.eng:
            for t in toks:
                S._wait(e, t)

    with cx.stack() as esg:
        msel = cx.sb(esg, [128, 2], F32, "msel")
        mB = Buf("msel")
        S.dma("sp", msel[:], msel_d, writes=[mB])
        for l in range(depth):
            P = dict(consts)
            P.update(_ab_decl(nc, "_%d" % l))
            P.update(abs_)
            if l == 0:
                P["xsrc"] = lambda t0: x_all0[t0:t0 + 128, :]
                P["xreads"] = []
            else:
                def xsrc_ab(t0):
                    r, rem = t0 // TOKC, t0 % TOKC
                    c, i0 = rem // XR, rem % XR
                    return xall[c][r * XR + i0:r * XR + i0 + 128, :]
                P["xsrc"] = xsrc_ab
                P["xreads"] = [xaB]

            def ystore(S_, col0, Yst, YB):
                for c in range(NYC):
                    S_.dma("sp", ysend[c][:, col0:col0 + 128].rearrange("(c p) e -> p c e", p=128),
                           Yst[:, c * (YR // 128):(c + 1) * (YR // 128), :], reads=[YB], writes=[ysB])
            P["ystore"] = ystore
            emit_ab(cx, P)
            gather(ysend, yall)

            Pc = {"c_rep": consts["c_rep"], "ident_f": consts["ident_f"], "fg_rep": fg_rep}
            Pc.update(_c_decl(nc, "_%d" % l))
            Pc.update(cs_)
            if l == 0:
                Pc["xsrc"] = lambda t0: x_half0[t0:t0 + 128, :]
                Pc["xreads"] = []
            else:
                Pc["xsrc"] = lambda t0: xloc[t0 // XR][t0 % XR:t0 % XR + 128, :]
                Pc["xreads"] = [xlB]
            cand = {}

            def c1_hook(cx_, es_, cand=cand):
                cand["r"] = cx_.sbring(es_, 2, [128, 2, D], BF16, "cand")

            def yload(S_, tt, yc, ycB, ycB2, cand=cand):
                ct, cB_ = cand["r"].next()
                xb = cand.setdefault(cB_.name, [Buf("c1"), Buf("c2"), Buf("c3")])
                bl = [cB_] + xb
                for hf in range(2):
                    t = hf * TOKC + tt * 128
                    c, i0 = t // YR, t % YR
                    S_.dma("sp", ct[:, hf, 0:1024], yall[c][i0:i0 + 128, :], reads=[yaB], writes=[bl[hf * 2]])
                    S_.dma("sp", ct[:, hf, 1024:2048], yall[c][YR + i0:YR + i0 + 128, :], reads=[yaB], writes=[bl[hf * 2 + 1]])
                S_.op("dve", lambda: nc.vector.tensor_scalar(out=yc[:], in0=ct[:, 0, :], scalar1=msel[:, 0:1], scalar2=None,
                                                             op0=ALU.mult), reads=bl + [mB], writes=[ycB])
                S_.op("dve", lambda: nc.vector.scalar_tensor_tensor(out=yc[:], in0=ct[:, 1, :], scalar=msel[:, 1:2], in1=yc[:],
                                                                    op0=ALU.mult, op1=ALU.add), reads=bl + [mB, ycB], writes=[ycB])
            Pc["c1_hook"] = c1_hook
            Pc["yload"] = yload
            if l == depth - 1:
                def ostore(S_, t0, xt, xB):
                    S_.dma("sp", out[t0:t0 + 128, :], xt[:], reads=[xB], writes=[outB])
            else:
                def ostore(S_, t0, xt, xB):
                    S_.dma("sp", xloc[t0 // XR][t0 % XR:t0 % XR + 128, :], xt[:], reads=[xB], writes=[xlB])
            Pc["ostore"] = ostore
            emit_c(cx, Pc, l == depth - 1)
            if l < depth - 1:
                gather(xloc, xall)
        S.barrier()
    for g in reversed(ccsems):
        g.__exit__(None, None, None)
    S.close()
    return nc


def kernel(**inputs):
    inp = {k: np.asarray(v) for k, v in inputs.items()}
    x = np.ascontiguousarray(inp["x"], dtype=np.float32)
    cores = list(range(8))
    if "fused" not in _PROG:
        _PROG["fused"] = build_fused()
    K = _consts()
    shared = [_c_shared(l, inp) for l in range(DEPTH)]
    in_maps = []
    for r in cores:
        b, hh = r // 2, r % 2
        m = {}
        for l in range(DEPTH):
            d = _ab_inputs(l, b, hh, None, inp)
            if l == 0:
                for k in AB_CONST_KEYS:
                    m[k] = d[k]
            for k in AB_LAYER_KEYS:
                m["%s_%d" % (k, l)] = d[k]
            for k in C_LAYER_KEYS:
                m["%s_%d" % (k, l)] = shared[l][k]
        m["fg_rep"] = shared[0]["fg_rep"]
        ms = np.zeros((128, 2), np.float32)
        ms[:, hh] = 1.0
        m["msel"] = ms
        m["x_all0"] = x[b]
        m["x_half0"] = np.ascontiguousarray(x[b, hh * TOKC:(hh + 1) * TOKC])
        in_maps.append(m)
    res = run_bass_kernel_spmd(_PROG["fused"], in_maps, core_ids=cores)
    outp = np.empty_like(x)
    for r in cores:
        outp[r // 2, (r % 2) * TOKC:(r % 2 + 1) * TOKC] = np.asarray(res.results[r]["out"])
    return outp


def kernel_unfused(**inputs):
    inp = {k: np.asarray(v) for k, v in inputs.items()}
    xcur = np.ascontiguousarray(inp["x"], dtype=np.float32)
    cores = list(range(8))
    for l in range(DEPTH):
        in_maps = [_ab_inputs(l, r // 2, r % 2, xcur[r // 2], inp) for r in cores]
        res = run_bass_kernel_spmd(_prog("ab"), in_maps, core_ids=cores)
        ys = [np.asarray(res.results[r]["y"]) for r in cores]
        shared = _c_shared(l, inp)
        in_maps = [_c_inputs(l, r // 2, r % 2, xcur[r // 2], (ys[(r // 2) * 2], ys[(r // 2) * 2 + 1]), inp, shared)
                   for r in cores]
        res = run_bass_kernel_spmd(_prog("c_last" if l == DEPTH - 1 else "c"), in_maps, core_ids=cores)
        xn = np.empty_like(xcur)
        for r in cores:
            xn[r // 2, (r % 2) * TOKC:(r % 2 + 1) * TOKC] = np.asarray(res.results[r]["out"])
        xcur = xn
    return xcur
```

```python
from contextlib import ExitStack
import numpy as np
import ml_dtypes
import concourse.bass as bass
import concourse.mybir as mybir
from concourse.bass_utils import run_bass_kernel_spmd

F32 = mybir.dt.float32
BF16 = mybir.dt.bfloat16
AF = mybir.ActivationFunctionType
ALU = mybir.AluOpType
AX = mybir.AxisListType

D = 2048
SEQ = 4096
NB = 4
DEPTH = 4
HD = 128
NCH = 32
KC = 16
TOKC = 2048
NEXP = 32
DEXP = 512
EPS = 1e-6
NEG = -1.0e30
ISQ = HD ** -0.5


class Buf:
    __slots__ = ("name", "w", "r")

    def __init__(self, name):
        self.name = name
        self.w = None
        self.r = {}


class Sched:
    NDMA = 8

    def __init__(self, nc):
        self.nc = nc
        self.eng = {"pe": nc.tensor, "dve": nc.vector, "act": nc.scalar,
                    "pool": nc.gpsimd, "sp": nc.sync}
        self.sem = {}
        self.cnt = {}
        self.waited = {}
        self._ctx = []
        for e in self.eng:
            g = nc.semaphore("s_" + e)
            self.sem[e] = g.__enter__()
            self._ctx.append(g)
            self.cnt[e] = 0
            self.waited[e] = {}
        self.dsem = {}
        self.dcnt = {}
        for q in ("sp", "act", "pool"):
            self.dsem[q] = []
            for i in range(self.NDMA):
                g = nc.semaphore("d_%s%d" % (q, i))
                self.dsem[q].append(g.__enter__())
                self._ctx.append(g)
            self.dcnt[q] = 0

    def close(self):
        for g in reversed(self._ctx):
            g.__exit__(None, None, None)

    def _wait(self, e, tok):
        if tok is None:
            return
        sem, val = tok
        if self.waited[e].get(sem.name, 0) >= val:
            return
        self.eng[e].wait_ge(sem, val)
        self.waited[e][sem.name] = val

    def _deps(self, e, reads, writes):
        for b in reads:
            self._wait(e, b.w)
        for b in writes:
            self._wait(e, b.w)
            for t in b.r.values():
                self._wait(e, t)

    def _mark(self, tok, reads, writes):
        for b in reads:
            b.r[tok[0].name] = tok
        for b in writes:
            b.w = tok
            b.r = {}

    def op(self, e, ins_fn, reads=(), writes=()):
        self._deps(e, reads, writes)
        ins = ins_fn()
        self.cnt[e] += 1
        ins.then_inc(self.sem[e], 1)
        tok = (self.sem[e], self.cnt[e])
        self._mark(tok, reads, writes)
        return tok

    def group(self, e, fns, reads=(), writes=()):
        self._deps(e, reads, writes)
        ins = None
        for f in fns:
            ins = f()
        self.cnt[e] += 1
        ins.then_inc(self.sem[e], 1)
        tok = (self.sem[e], self.cnt[e])
        self._mark(tok, reads, writes)
        return tok

    def dma(self, q, out, in_, reads=(), writes=()):
        i = self.dcnt[q]
        self.dcnt[q] += 1
        sem = self.dsem[q][i % self.NDMA]
        rnd = i // self.NDMA
        if rnd > 0:
            self._wait(q, (sem, 16 * rnd))
        self._deps(q, reads, writes)
        ins = self.eng[q].dma_start(out=out, in_=in_)
        ins.then_inc(sem, 16)
        tok = (sem, 16 * (rnd + 1))
        self._mark(tok, reads, writes)
        return tok

    def new_epoch(self):
        for e in ("pe", "dve", "act"):
            if self.cnt[e] == 0:
                continue
            g = self.nc.semaphore("s_%s_e%d" % (e, len(self._ctx)))
            self.sem[e] = g.__enter__()
            self._ctx.append(g)
            self.cnt[e] = 0

    def all_tokens(self):
        toks = []
        for e in self.eng:
            if self.cnt[e] > 0:
                toks.append((self.sem[e], self.cnt[e]))
        for q in self.dsem:
            n = self.dcnt[q]
            for j in range(self.NDMA):
                if n > j:
                    uses = (n - 1 - j) // self.NDMA + 1
                    toks.append((self.dsem[q][j], 16 * uses))
        return toks

    def barrier(self, engines=None):
        toks = self.all_tokens()
        for e in (engines or self.eng):
            for t in toks:
                self._wait(e, t)


class Ring:
    def __init__(self, tiles, name):
        self.tiles = tiles
        self.bufs = [Buf("%s%d" % (name, i)) for i in range(len(tiles))]
        self.i = 0

    def next(self):
        j = self.i % len(self.tiles)
        self.i += 1
        return self.tiles[j], self.bufs[j]


class Ctx:
    def __init__(self, nc):
        self.nc = nc
        self.S = Sched(nc)
        self.uid = 0

    def stack(self):
        return ExitStack()

    def sb(self, es, shape, dt, name=None):
        self.uid += 1
        return es.enter_context(self.nc.sbuf_tensor("%s_%d" % (name or "t", self.uid), list(shape), dt))

    def ps(self, es, shape, dt, name=None):
        self.uid += 1
        return es.enter_context(self.nc.psum_tensor("%s_%d" % (name or "p", self.uid), list(shape), dt))

    def sbring(self, es, n, shape, dt, name):
        return Ring([self.sb(es, shape, dt, name) for _ in range(n)], name)

    def psring(self, es, n, shape, dt, name):
        return Ring([self.ps(es, shape, dt, name) for _ in range(n)], name)


def emit_mod_cols(cx, es, adaw_src, cact, cactB, adab_col, out_col, out_B, ncols_chunks,
                  wring, psum, psB):
    nc, S = cx.nc, cx.S
    npieces = ncols_chunks // 4
    for pc in range(npieces):
        wt, wb = wring.next()
        S.dma("pool", wt[:], adaw_src[:, pc * 512:(pc + 1) * 512].rearrange("(k p) n -> p k n", p=128),
              writes=[wb])
        for c4 in range(4):
            cc = pc * 4 + c4
            fns = []
            for k in range(KC):
                fns.append(lambda k=k, c4=c4, cc=cc, wt=wt: nc.tensor.matmul(
                    psum[:, cc:cc + 1], wt[:, k, c4 * 128:(c4 + 1) * 128], cact[:, k, 0:1],
                    start=(k == 0), stop=(k == KC - 1)))
            S.group("pe", fns, reads=[wb, cactB], writes=[psB])
    S.op("dve", lambda: nc.vector.tensor_tensor(out=out_col[:], in0=psum[:, 0:ncols_chunks], in1=adab_col[:],
                                                op=ALU.add), reads=[psB], writes=[out_B])


def emit_cact(cx, es, c_rep_dram, cact, cactB, tmp):
    nc, S = cx.nc, cx.S
    tB = Buf("ctmp")
    S.dma("sp", tmp[:], c_rep_dram, writes=[tB])
    S.op("act", lambda: nc.scalar.activation(out=cact[:], in_=tmp[:], func=AF.Silu), reads=[tB], writes=[cactB])


def emit_rms_stats(cx, xt, xB, junk, junkB, ss, ssB, epsc):
    nc, S = cx.nc, cx.S
    S.op("act", lambda: nc.scalar.activation(out=junk[:], in_=xt[:], func=AF.Square, accum_out=ss[:, 0:1]),
         reads=[xB], writes=[junkB, ssB])
    S.op("act", lambda: nc.scalar.activation(out=ss[:, 1:2], in_=ss[:, 0:1], func=AF.Sqrt, scale=1.0 / D,
                                             bias=epsc[:, 0:1]), reads=[ssB], writes=[ssB])
    S.op("dve", lambda: nc.vector.reciprocal(out=ss[:, 2:3], in_=ss[:, 1:2]), reads=[ssB], writes=[ssB])


NFM = 18
NTM = 1536


def _ab_decl(nc, sfx=""):
    def din(name, shape, dt=F32):
        return nc.dram_tensor(name + sfx, list(shape), dt, kind="ExternalInput").ap()
    P = {}
    P["adaw"] = din("adaw", [D, 2 * D])
    P["adab_col"] = din("adab_col", [128, 32])
    P["g1col"] = din("g1col", [128, KC])
    P["w_fm"] = din("w_fm", [D, NFM * 128])
    P["w_tm"] = din("w_tm", [D, NTM])
    P["w_g"] = din("w_g", [D, 8])
    P["gn_rep"] = din("gn_rep", [128, 4, 128])
    P["conv_cols"] = din("conv_cols", [128, 2, 5])
    P["wq"] = din("wq", [2, 128, 128])
    P["wk"] = din("wk", [2, 128, 128])
    P["gate_b"] = din("gate_b", [4, 3])
    return P


def _ab_const_decl(nc):
    def din(name, shape, dt=F32):
        return nc.dram_tensor(name, list(shape), dt, kind="ExternalInput").ap()
    P = {}
    P["c_rep"] = din("c_rep", [128, KC, 128])
    P["ident_f"] = din("ident_f", [128, 128])
    P["rotC"] = din("rotC", [128, SEQ])
    P["rotS"] = din("rotS", [128, SEQ])
    P["ret_DT"] = din("ret_DT", [128, 2, 128])
    P["ret_cols"] = din("ret_cols", [128, 2, 3])
    P["rows_c"] = din("rows_c", [4, 3, SEQ])
    P["mask_m"] = din("mask_m", [128, 128])
    P["mask_d"] = din("mask_d", [128, 128])
    P["onehot"] = din("onehot", [4, 4, 128])
    return P


def _ab_scratch(nc):
    P = {}
    P["FMs"] = nc.dram_tensor("FMs", [NFM, 128, SEQ], BF16, kind="Internal").ap()
    P["TMs"] = nc.dram_tensor("TMs", [SEQ, NTM], BF16, kind="Internal").ap()
    P["Gs"] = nc.dram_tensor("Gs", [8, SEQ], F32, kind="Internal").ap()
    P["FMsB"], P["TMsB"], P["GsB"] = Buf("FMs"), Buf("TMs"), Buf("Gs")
    return P


def build_ab():
    nc = bass.Bass("TRN2", target_bir_lowering=False)
    cx = Ctx(nc)
    P = {}
    P.update(_ab_const_decl(nc))
    P.update(_ab_decl(nc))
    P.update(_ab_scratch(nc))
    x = nc.dram_tensor("x", [SEQ, D], F32, kind="ExternalInput").ap()
    y_out = nc.dram_tensor("y", [SEQ, 1024], BF16, kind="ExternalOutput").ap()
    yB = Buf("y")
    P["xsrc"] = lambda t0: x[t0:t0 + 128, :]
    P["xreads"] = []

    def ystore(S, col0, Yst, YB):
        S.dma("sp", y_out[:, col0:col0 + 128].rearrange("(c p) e -> p c e", p=128), Yst[:], reads=[YB], writes=[yB])
    P["ystore"] = ystore
    emit_ab(cx, P)
    cx.S.close()
    return nc


def emit_ab(cx, P):
    nc = cx.nc
    S = cx.S
    c_rep, adaw, adab_col, g1col = P["c_rep"], P["adaw"], P["adab_col"], P["g1col"]
    w_fm, w_tm, w_g, ident_f, rotC, rotS = P["w_fm"], P["w_tm"], P["w_g"], P["ident_f"], P["rotC"], P["rotS"]
    ret_DT, ret_cols, gn_rep, conv_cols = P["ret_DT"], P["ret_cols"], P["gn_rep"], P["conv_cols"]
    wq, wk, gate_b, rows_c, mask_m, mask_d, onehot = (P["wq"], P["wk"], P["gate_b"], P["rows_c"], P["mask_m"],
                                                      P["mask_d"], P["onehot"])
    FMs, TMs, Gs, FMsB, TMsB, GsB = P["FMs"], P["TMs"], P["Gs"], P["FMsB"], P["TMsB"], P["GsB"]

    with cx.stack() as es0:
        identf = cx.sb(es0, [128, 128], F32, "identf")
        identb = cx.sb(es0, [128, 128], BF16, "identb")
        epsc = cx.sb(es0, [128, 1], F32, "epsc")
        cB = Buf("consts")
        S.dma("sp", identf[:], ident_f, writes=[cB])
        S.op("dve", lambda: nc.vector.tensor_copy(out=identb[:], in_=identf[:]), reads=[cB], writes=[cB])
        S.op("dve", lambda: nc.vector.memset(epsc[:], EPS), writes=[cB])
        scale_col = cx.sb(es0, [128, KC], F32, "scalecol")
        shift_col = cx.sb(es0, [128, KC], F32, "shiftcol")
        modB = Buf("mod")

        with cx.stack() as es:
            cact = cx.sb(es, [128, KC, 128], BF16, "cact")
            cactB = Buf("cact")
            ctmp = cx.sb(es, [128, KC, 128], F32, "ctmp")
            emit_cact(cx, es, c_rep, cact, cactB, ctmp)
            wring = cx.sbring(es, 3, [128, KC, 512], BF16, "wp")
            modps = cx.ps(es, [128, 32], F32, "modps")
            modpsB = Buf("modps")
            modcol = cx.sb(es, [128, 32], F32, "modcol")
            adabc = cx.sb(es, [128, 32], F32, "adabc")
            g1c = cx.sb(es, [128, KC], F32, "g1c")
            aB = Buf("adab")
            S.dma("sp", adabc[:], adab_col, writes=[aB])
            S.dma("sp", g1c[:], g1col, writes=[aB])
            emit_mod_cols(cx, es, adaw, cact, cactB, adabc, modcol, modB, 32, wring, modps, modpsB)
            S.op("dve", lambda: nc.vector.tensor_copy(out=shift_col[:], in_=modcol[:, 0:KC]), reads=[modB], writes=[modB])
            S.op("dve", lambda: nc.vector.scalar_tensor_tensor(out=scale_col[:], in0=modcol[:, KC:2 * KC], scalar=1.0,
                                                               in1=g1c[:], op0=ALU.add, op1=ALU.mult),
                 reads=[modB, aB], writes=[modB])

            wg_t = cx.sb(es, [128, KC, 8], BF16, "wg")
            wgB = Buf("wg")
            S.dma("pool", wg_t[:], w_g.rearrange("(k p) n -> p k n", p=128), writes=[wgB])
            xring = cx.sbring(es, 4, [128, D], F32, "xt")
            xnring = cx.sbring(es, 2, [128, D], BF16, "xn")
            junk = cx.sb(es, [128, D], BF16, "junk")
            junkB = Buf("junk")
            ssring = cx.sbring(es, 2, [128, 4], F32, "ss")
            tpring = cx.psring(es, 2, [128, KC, 128], BF16, "tp")
            hring = cx.sbring(es, 2, [128, KC, 512], BF16, "hT")
            mmring = cx.psring(es, 3, [128, 512], F32, "mm")
            stg_b = cx.sbring(es, 3, [128, 512], BF16, "stgb")
            stg_f = cx.sbring(es, 2, [8, 512], F32, "stgf")
            pieces = [("fm", p * 512, min(512, NFM * 128 - p * 512)) for p in range((NFM * 128 + 511) // 512)]
            pieces += [("tm", p * 512, 512) for p in range(NTM // 512)]
            evac_i = 0
            pre = {}

            def load_x(tb_):
                for tt_ in range(4):
                    t0_ = tb_ * 512 + tt_ * 128
                    xt_, xB_ = xring.next()
                    S.dma("sp", xt_[:], P["xsrc"](t0_), reads=P["xreads"], writes=[xB_])
                    pre[(tb_, tt_)] = (xt_, xB_)
            load_x(0)
            for tb in range(SEQ // 512):
                hT, hB = hring.next()
                for tt in range(4):
                    t0 = tb * 512 + tt * 128
                    xt, xB = pre.pop((tb, tt))
                    ss, ssB = ssring.next()
                    emit_rms_stats(cx, xt, xB, junk, junkB, ss, ssB, epsc)
                    xn, xnB = xnring.next()
                    S.op("dve", lambda: nc.vector.tensor_scalar(out=xn[:], in0=xt[:], scalar1=ss[:, 2:3], scalar2=None,
                                                                op0=ALU.mult), reads=[xB, ssB], writes=[xnB])
                    tp, tpB = tpring.next()
                    S.group("pe", [lambda k=k: nc.tensor.transpose(tp[:, k, :], xn[:, k * 128:(k + 1) * 128], identb[:])
                                   for k in range(KC)], reads=[xnB, cB], writes=[tpB])
                    for k in range(KC):
                        S.op("act", lambda k=k: nc.scalar.activation(
                            out=hT[:, k, tt * 128:(tt + 1) * 128], in_=tp[:, k, :], func=AF.Identity,
                            scale=scale_col[:, k:k + 1], bias=shift_col[:, k:k + 1]),
                             reads=[tpB, modB], writes=[hB])
                if tb + 1 < SEQ // 512:
                    load_x(tb + 1)
                mm, mmB = mmring.next()
                S.group("pe", [lambda k=k: nc.tensor.matmul(mm[0:8, :], wg_t[:, k, :], hT[:, k, :],
                                                            start=(k == 0), stop=(k == KC - 1)) for k in range(KC)],
                        reads=[wgB, hB], writes=[mmB])
                sf, sfB = stg_f.next()
                S.op("dve", lambda: nc.vector.tensor_copy(out=sf[:], in_=mm[0:8, :]), reads=[mmB], writes=[sfB])
                S.dma("sp", Gs[:, tb * 512:(tb + 1) * 512], sf[:], reads=[sfB], writes=[GsB])
                for (kind, c0, ncol) in pieces:
                    wt, wb = wring.next()
                    src = w_fm if kind == "fm" else w_tm
                    S.dma("pool", wt[:, :, 0:ncol], src[:, c0:c0 + ncol].rearrange("(k p) n -> p k n", p=128),
                          writes=[wb])
                    if kind == "fm":
                        for c4 in range(ncol // 128):
                            ch = c0 // 128 + c4
                            mm, mmB = mmring.next()
                            S.group("pe", [lambda k=k, c4=c4, mm=mm, wt=wt: nc.tensor.matmul(
                                mm[:, :], wt[:, k, c4 * 128:(c4 + 1) * 128], hT[:, k, :],
                                start=(k == 0), stop=(k == KC - 1)) for k in range(KC)],
                                    reads=[wb, hB], writes=[mmB])
                            sg, sgB = stg_b.next()
                            eng = "act" if (evac_i % 2 == 0) else "dve"
                            evac_i += 1
                            if eng == "act":
                                S.op("act", lambda: nc.scalar.copy(out=sg[:], in_=mm[:, :]), reads=[mmB], writes=[sgB])
                            else:
                                S.op("dve", lambda: nc.vector.tensor_copy(out=sg[:], in_=mm[:, :]), reads=[mmB], writes=[sgB])
                            S.dma("sp", FMs[ch, :, tb * 512:(tb + 1) * 512], sg[:], reads=[sgB], writes=[FMsB])
                    else:
                        for tt in range(4):
                            mm, mmB = mmring.next()
                            S.group("pe", [lambda k=k, tt=tt, mm=mm, wt=wt: nc.tensor.matmul(
                                mm[:, :], hT[:, k, tt * 128:(tt + 1) * 128], wt[:, k, :],
                                start=(k == 0), stop=(k == KC - 1)) for k in range(KC)],
                                    reads=[wb, hB], writes=[mmB])
                            sg, sgB = stg_b.next()
                            eng = "act" if (evac_i % 2 == 0) else "dve"
                            evac_i += 1
                            if eng == "act":
                                S.op("act", lambda: nc.scalar.copy(out=sg[:], in_=mm[:, :]), reads=[mmB], writes=[sgB])
                            else:
                                S.op("dve", lambda: nc.vector.tensor_copy(out=sg[:], in_=mm[:, :]), reads=[mmB], writes=[sgB])
                            t0 = tb * 512 + tt * 128
                            S.dma("sp", TMs[t0:t0 + 128, c0:c0 + 512], sg[:], reads=[sgB], writes=[TMsB])
            S.barrier()

        es2 = es0
        tab = cx.sb(es2, [128, 4, NCH, NCH], F32, "tab")
        colsM = cx.sb(es2, [128, NCH, 8], F32, "colsM")
        sbM = cx.sb(es2, [128, 2, 64], F32, "sbM")
        tabB, colsB, sbB = Buf("tab"), Buf("colsM"), Buf("sbM")
        with cx.stack() as es:
            rows = cx.sb(es, [4, 3, SEQ], F32, "rows")
            gb = cx.sb(es, [4, 3], F32, "gb")
            ngb = cx.sb(es, [4, 3], F32, "ngb")
            one4 = cx.sb(es, [4, 1], F32, "one4")
            oh = cx.sb(es, [4, 4, 128], F32, "oh")
            rB = Buf("rows")
            S.dma("sp", rows[:], rows_c, writes=[rB])
            S.dma("sp", gb[:], gate_b, writes=[rB])
            S.dma("sp", oh[:], onehot, writes=[rB])
            S.op("dve", lambda: nc.vector.tensor_scalar(out=ngb[:], in0=gb[:], scalar1=-1.0, scalar2=None, op0=ALU.mult),
                 reads=[rB], writes=[rB])
            S.op("dve", lambda: nc.vector.memset(one4[:], 1.0), writes=[rB])
            pt = cx.ps(es, [128, NCH, 8], F32, "pt")
            ptB = Buf("pt")
            pb = cx.ps(es, [128, 4, 64], F32, "pb")
            pbB = Buf("pb")
            wB = Buf("work")
            with cx.stack() as esf:
                gff = cx.sb(esf, [4, SEQ], F32, "gff")
                A = cx.sb(esf, [4, SEQ], F32, "A")
                gB = Buf("g")
                S.dma("sp", gff[:], Gs[4:8, :], reads=[GsB], writes=[gB])
                S.op("act", lambda: nc.scalar.activation(out=gff[:], in_=gff[:], func=AF.Exp, scale=-1.0, bias=ngb[:, 2:3]),
                     reads=[gB, rB], writes=[gB])
                S.op("act", lambda: nc.scalar.activation(out=gff[:], in_=gff[:], func=AF.Ln, scale=1.0, bias=one4[:, 0:1]),
                     reads=[gB, rB], writes=[gB])
                S.op("dve", lambda: nc.vector.tensor_tensor_scan(out=A[:], data0=rows[:, 0, :], data1=gff[:], initial=0.0,
                                                                 op0=ALU.mult, op1=ALU.add), reads=[gB, rB], writes=[wB])
                S.group("pe", [lambda c=c: nc.tensor.transpose(pt[:, c, 0:4], A[:, c * 128:(c + 1) * 128], identf[0:4, 0:4])
                               for c in range(NCH)], reads=[wB, cB], writes=[ptB])
                AT = cx.sb(esf, [128, NCH, 4], F32, "AT")
                AendB_t = cx.sb(esf, [128, 4, NCH], F32, "AendB")
                S.op("dve", lambda: nc.vector.tensor_copy(out=AT[:], in_=pt[:, :, 0:4]), reads=[ptB], writes=[wB])
                Aend = cx.sb(esf, [4, NCH], F32, "Aend")
                S.op("dve", lambda: nc.vector.tensor_copy(out=Aend[:], in_=A[:].rearrange("h (c l) -> h c l", l=128)[:, :, 127]),
                     reads=[wB], writes=[wB])
                S.group("pe", [lambda h=h: nc.tensor.matmul(pb[:, h, 0:NCH], oh[:, h, :], Aend[:], start=True, stop=True)
                               for h in range(4)], reads=[wB, rB], writes=[pbB])
                S.op("dve", lambda: nc.vector.tensor_copy(out=AendB_t[:], in_=pb[:, :, 0:NCH]), reads=[pbB], writes=[wB])
                for h in range(4):
                    for qb in range(NCH):
                        S.op("dve", lambda h=h, qb=qb: nc.vector.tensor_scalar(
                            out=tab[:, h, qb, :], in0=AT[:, :, h], scalar1=AendB_t[:, h, qb:qb + 1], scalar2=None,
                            op0=ALU.subtract), reads=[wB], writes=[tabB])
                S.barrier()
            with cx.stack() as esm:
                g = cx.sb(esm, [2, SEQ], F32, "g")
                Lf = cx.sb(esm, [2, SEQ], F32, "Lf")
                Bc = cx.sb(esm, [2, SEQ], F32, "Bc")
                M = cx.sb(esm, [2, SEQ], F32, "M")
                mm_ = cx.sb(esm, [2, SEQ], F32, "mm")
                sm = cx.sb(esm, [2, 8, NCH], F32, "sm")
                mB = Buf("mwork")
                S.dma("sp", g[:], Gs[0:2, :], reads=[GsB], writes=[mB])
                S.dma("sp", Lf[:], Gs[2:4, :], reads=[GsB], writes=[mB])
                S.op("act", lambda: nc.scalar.activation(out=Lf[:], in_=Lf[:], func=AF.Exp, scale=-1.0, bias=ngb[0:2, 1:2]),
                     reads=[mB, rB], writes=[mB])
                S.op("act", lambda: nc.scalar.activation(out=Lf[:], in_=Lf[:], func=AF.Ln, scale=1.0, bias=one4[0:2, 0:1]),
                     reads=[mB, rB], writes=[mB])
                S.op("dve", lambda: nc.vector.tensor_tensor_scan(out=Bc[:], data0=rows[0:2, 1, :], data1=Lf[:], initial=0.0,
                                                                 op0=ALU.mult, op1=ALU.add), reads=[mB, rB], writes=[mB])
                S.op("dve", lambda: nc.vector.scalar_tensor_tensor(out=g[:], in0=g[:], scalar=gb[0:2, 0:1], in1=Bc[:],
                                                                   op0=ALU.add, op1=ALU.add), reads=[mB, rB], writes=[mB])
                S.op("dve", lambda: nc.vector.tensor_tensor_scan(out=M[:], data0=rows[0:2, 2, :], data1=g[:], initial=NEG,
                                                                 op0=ALU.add, op1=ALU.max), reads=[mB, rB], writes=[mB])
                Mv = M[:].rearrange("h (c l) -> h c l", l=128)
                Bv = Bc[:].rearrange("h (c l) -> h c l", l=128)
                S.op("dve", lambda: nc.vector.tensor_copy(out=sm[:, 0, :], in_=Mv[:, :, 127]), reads=[mB], writes=[mB])
                S.op("dve", lambda: nc.vector.tensor_copy(out=sm[:, 1, :], in_=Bv[:, :, 127]), reads=[mB], writes=[mB])
                S.op("dve", lambda: nc.vector.tensor_tensor_scan(out=sm[:, 2, :], data0=sm[:, 0, :], data1=sm[:, 1, :],
                                                                 initial=NEG, op0=ALU.max, op1=ALU.subtract),
                     reads=[mB], writes=[mB])
                S.op("dve", lambda: nc.vector.memset(sm[:, 3, 0:1], NEG), reads=[mB], writes=[mB])
                S.op("dve", lambda: nc.vector.tensor_copy(out=sm[:, 3, 1:NCH], in_=sm[:, 2, 0:NCH - 1]), reads=[mB], writes=[mB])
                S.op("dve", lambda: nc.vector.tensor_tensor(out=sm[:, 4, :], in0=sm[:, 3, :], in1=sm[:, 0, :], op=ALU.max),
                     reads=[mB], writes=[mB])
                S.op("dve", lambda: nc.vector.tensor_tensor(out=sm[:, 5, :], in0=sm[:, 3, :], in1=sm[:, 4, :], op=ALU.subtract),
                     reads=[mB], writes=[mB])
                S.op("dve", lambda: nc.vector.tensor_tensor(out=sm[:, 6, :], in0=sm[:, 0, :], in1=sm[:, 4, :], op=ALU.subtract),
                     reads=[mB], writes=[mB])
                S.op("act", lambda: nc.scalar.activation(out=sm[:, 5:7, :], in_=sm[:, 5:7, :], func=AF.Exp),
                     reads=[mB], writes=[mB])
                for c in range(NCH):
                    sl = slice(c * 128, (c + 1) * 128)
                    S.op("dve", lambda c=c, sl=sl: nc.vector.tensor_scalar(out=mm_[:, sl], in0=M[:, sl], scalar1=sm[:, 3, c:c + 1],
                                                                           scalar2=None, op0=ALU.max), reads=[mB], writes=[mB])
                    S.op("dve", lambda c=c, sl=sl: nc.vector.tensor_scalar(out=g[:, sl], in0=g[:, sl], scalar1=sm[:, 0, c:c + 1],
                                                                           scalar2=None, op0=ALU.subtract), reads=[mB], writes=[mB])
                    S.op("dve", lambda c=c, sl=sl: nc.vector.tensor_scalar(out=M[:, sl], in0=mm_[:, sl], scalar1=sm[:, 0, c:c + 1],
                                                                           scalar2=-1.0, op0=ALU.subtract, op1=ALU.mult),
                         reads=[mB], writes=[mB])
                S.op("dve", lambda: nc.vector.tensor_tensor(out=Bc[:], in0=Bc[:], in1=mm_[:], op=ALU.subtract),
                     reads=[mB], writes=[mB])
                for c in range(NCH):
                    sl = slice(c * 128, (c + 1) * 128)
                    S.op("dve", lambda c=c, sl=sl: nc.vector.tensor_scalar(out=mm_[:, sl], in0=mm_[:, sl], scalar1=sm[:, 3, c:c + 1],
                                                                           scalar2=-1.0, op0=ALU.subtract, op1=ALU.mult),
                         reads=[mB], writes=[mB])
                qs = [g, M, mm_, Bc]
                for qt in qs:
                    S.op("act", lambda qt=qt: nc.scalar.activation(out=qt[:], in_=qt[:], func=AF.Exp), reads=[mB], writes=[mB])
                fns = []
                for c in range(NCH):
                    for q in range(4):
                        fns.append(lambda c=c, q=q: nc.tensor.transpose(pt[:, c, q * 2:q * 2 + 2], qs[q][:, c * 128:(c + 1) * 128],
                                                                        identf[0:2, 0:2]))
                S.group("pe", fns, reads=[mB, cB, wB], writes=[ptB])
                S.op("dve", lambda: nc.vector.tensor_copy(out=colsM[:], in_=pt[:]), reads=[ptB], writes=[colsB])
                S.group("pe", [lambda h=h: nc.tensor.matmul(pb[:, h, :], oh[0:2, h, :],
                                                            sm[:, 5:7, :].rearrange("h a c -> h (a c)"), start=True, stop=True)
                               for h in range(2)], reads=[mB, rB, wB], writes=[pbB])
                S.op("dve", lambda: nc.vector.tensor_copy(out=sbM[:], in_=pb[:, 0:2, :]), reads=[pbB], writes=[sbB])
                S.barrier()

        with cx.stack() as es:
            QT = cx.sb(es, [128, SEQ], BF16, "QT")
            KT = cx.sb(es, [128, SEQ], BF16, "KT")
            Ktok = cx.sb(es, [128, NCH, 128], BF16, "Ktok")
            V1 = cx.sb(es, [128, NCH, 129], BF16, "V1")
            GT = cx.sb(es, [128, NCH, 128], BF16, "GT")
            Yst = cx.sb(es, [128, NCH, 128], BF16, "Yst")
            gnr = cx.sb(es, [128, 4, 128], F32, "gnr")
            f32a = cx.sb(es, [128, SEQ], F32, "f32a")
            f32b = cx.sb(es, [128, SEQ], F32, "f32b")
            ldq = cx.sb(es, [128, SEQ], BF16, "ldq")
            ldp = cx.sb(es, [128, SEQ], BF16, "ldp")
            rc = cx.sb(es, [128, SEQ], F32, "rotC")
            rs = cx.sb(es, [128, SEQ], F32, "rotS")
            rDT = cx.sb(es, [128, 2, 128], F32, "rDT")
            rcols = cx.sb(es, [128, 2, 3], F32, "rcols")
            ccols = cx.sb(es, [128, 2, 5], F32, "ccols")
            wqk = cx.sb(es, [128, 4, 128], BF16, "wqk")
            mskm = cx.sb(es, [128, 128], F32, "mskm")
            Rst = cx.sb(es, [128, 129], F32, "Rst")
            Rbf = cx.sb(es, [128, 129], BF16, "Rbf")
            kB = Buf("hconst")
            S.dma("sp", gnr[:], gn_rep, writes=[kB])
            S.dma("sp", rc[:], rotC, writes=[kB])
            S.dma("sp", rs[:], rotS, writes=[kB])
            S.dma("sp", rDT[:], ret_DT, writes=[kB])
            S.dma("sp", rcols[:], ret_cols, writes=[kB])
            S.dma("sp", ccols[:], conv_cols, writes=[kB])
            S.dma("sp", mskm[:], mask_m, writes=[kB])
            for i in range(2):
                S.dma("pool", wqk[:, i, :], wq[i], writes=[kB])
                S.dma("pool", wqk[:, 2 + i, :], wk[i], writes=[kB])
            QB, KB_, KtB, VB, GB, YB, RB = Buf("QT"), Buf("KT"), Buf("Ktok"), Buf("V1"), Buf("GT"), Buf("Yst"), Buf("R")
            lB, fB = Buf("ld"), Buf("f32")
            ps_s = cx.psring(es, 2, [128, 128], F32, "ps_s")
            ps_o = cx.psring(es, 1, [128, 256], F32, "ps_o")
            ps_x = cx.psring(es, 1, [128, 256], F32, "ps_x")
            ps_kv = cx.psring(es, 1, [128, 256], F32, "ps_kv")
            ps_t = cx.psring(es, 1, [128, 512], BF16, "ps_t")
            psq_ring = cx.psring(es, 2, [128, 512], F32, "psq")
            PTr = cx.sbring(es, 2, [128, 128], BF16, "PT")
            Vzr = cx.sbring(es, 2, [128, 129], BF16, "Vz")
            t1r = cx.sbring(es, 2, [128, 129], F32, "t1")
            t2r = cx.sbring(es, 2, [128, 129], F32, "t2")
            Rbr = cx.sbring(es, 2, [128, 129], BF16, "Rbr")
            ndr = cx.sbring(es, 2, [128, 132], F32, "nd")
            str_ = cx.sbring(es, 2, [128, 8], F32, "st")
            hnr = cx.sbring(es, 2, [128, 128], F32, "hn")
            sgr = cx.sbring(es, 2, [128, 128], F32, "sg")
            bnr = cx.sbring(es, 2, [128, 8], F32, "bn")

            def load_tok(dst, dstB, col0, width=128):
                S.dma("sp", dst[:, :, 0:width], TMs[:, col0:col0 + width].rearrange("(c p) e -> p c e", p=128),
                      reads=[TMsB], writes=[dstB])

            def groupnorm_gate(o_ap, oB, gate_func, gi_idx, c):
                bn, bnB = bnr.next()
                S.op("dve", lambda: nc.vector.bn_stats(out=bn[:, 0:6], in_=o_ap), reads=[oB], writes=[bnB])
                S.op("dve", lambda: nc.vector.bn_aggr(out=bn[:, 6:8], in_=bn[:, 0:6]), reads=[bnB], writes=[bnB])
                st, stB = str_.next()
                S.op("act", lambda: nc.scalar.activation(out=st[:, 0:1], in_=bn[:, 7:8], func=AF.Sqrt, scale=1.0,
                                                         bias=epsc[:, 0:1]), reads=[bnB, cB], writes=[stB])
                S.op("dve", lambda: nc.vector.reciprocal(out=st[:, 1:2], in_=st[:, 0:1]), reads=[stB], writes=[stB])
                hn, hnB = hnr.next()
                S.op("dve", lambda: nc.vector.tensor_scalar(out=hn[:], in0=o_ap, scalar1=bn[:, 6:7], scalar2=st[:, 1:2],
                                                            op0=ALU.subtract, op1=ALU.mult), reads=[oB, bnB, stB], writes=[hnB])
                sg, sgB = sgr.next()
                S.op("act", lambda: nc.scalar.activation(out=sg[:], in_=GT[:, c, :], func=gate_func), reads=[GB], writes=[sgB])
                S.op("dve", lambda: nc.vector.tensor_tensor(out=hn[:], in0=hn[:], in1=gnr[:, gi_idx, :], op=ALU.mult),
                     reads=[hnB, kB], writes=[hnB])
                S.op("dve", lambda: nc.vector.tensor_tensor(out=Yst[:, c, :], in0=hn[:], in1=sg[:], op=ALU.mult),
                     reads=[hnB, sgB], writes=[YB])

            for hh in range(2):
                for (dst, dstB, ch, chp) in ((QT, QB, 8 + hh, 12 + hh), (KT, KB_, 10 + hh, 14 + hh)):
                    S.dma("sp", ldq[:], FMs[ch], reads=[FMsB], writes=[lB])
                    S.dma("sp", ldp[:], FMs[chp], reads=[FMsB], writes=[lB])
                    S.op("dve", lambda: nc.vector.tensor_tensor(out=f32a[:], in0=ldq[:], in1=rc[:], op=ALU.mult),
                         reads=[lB, kB], writes=[fB])
                    S.op("dve", lambda: nc.vector.tensor_tensor(out=f32b[:], in0=ldp[:], in1=rs[:], op=ALU.mult),
                         reads=[lB, kB], writes=[fB])
                    S.op("dve", lambda dst=dst: nc.vector.tensor_tensor(out=dst[:], in0=f32a[:], in1=f32b[:], op=ALU.add),
                         reads=[fB], writes=[dstB])
                for c4 in range(NCH // 4):
                    pt_, ptB_ = ps_t.next()
                    S.group("pe", [lambda j=j, c4=c4: nc.tensor.transpose(pt_[:, j * 128:(j + 1) * 128],
                                                                          KT[:, (c4 * 4 + j) * 128:(c4 * 4 + j + 1) * 128], identb[:])
                                   for j in range(4)], reads=[KB_, cB], writes=[ptB_])
                    S.op("act", lambda c4=c4: nc.scalar.copy(out=Ktok[:, c4 * 4:(c4 + 1) * 4, :],
                                                             in_=pt_[:, :].rearrange("p (j e) -> p j e", e=128)),
                         reads=[ptB_], writes=[KtB])
                load_tok(V1, VB, hh * 128)
                load_tok(GT, GB, 256 + hh * 128)
                S.op("dve", lambda: nc.vector.memset(Rst[:], 0.0), writes=[RB])
                rb0, rb0B = Rbr.next()
                S.op("dve", lambda: nc.vector.memset(rb0[:], 0.0), writes=[rb0B])
                rcur = [rb0, rb0B]

                def ret_A(c):
                    sl = slice(c * 128, (c + 1) * 128)
                    pss, pssB = ps_s.next()
                    S.op("pe", lambda: nc.tensor.matmul(pss[:], KT[:, sl], QT[:, sl], start=True, stop=True),
                         reads=[KB_, QB], writes=[pssB])
                    PT, PTB = PTr.next()
                    S.op("dve", lambda: nc.vector.tensor_tensor(out=PT[:], in0=pss[:], in1=rDT[:, hh, :], op=ALU.mult),
                         reads=[pssB, kB], writes=[PTB])
                    pso, psoB = ps_o.next()
                    S.op("pe", lambda: nc.tensor.matmul(pso[:, 0:128], PT[:], V1[:, c, 0:128], start=True, stop=True),
                         reads=[PTB, VB], writes=[psoB])
                    t1, t1B = t1r.next()
                    S.op("act", lambda: nc.scalar.copy(out=t1[:, 0:128], in_=pso[:, 0:128]), reads=[psoB], writes=[t1B])
                    Vz, VzB = Vzr.next()
                    S.op("dve", lambda: nc.vector.tensor_scalar(out=Vz[:, 0:128], in0=V1[:, c, 0:128], scalar1=rcols[:, hh, 0:1],
                                                                scalar2=None, op0=ALU.mult), reads=[VB, kB], writes=[VzB])
                    pkv, pkvB = ps_kv.next()
                    S.op("pe", lambda: nc.tensor.matmul(pkv[:, 0:128], Ktok[:, c, :], Vz[:, 0:128], start=True, stop=True),
                         reads=[KtB, VzB], writes=[pkvB])
                    t2, t2B = t2r.next()
                    S.op("act", lambda: nc.scalar.copy(out=t2[:, 0:128], in_=pkv[:, 0:128]), reads=[pkvB], writes=[t2B])
                    return (t1, t1B, t2, t2B)

                def ret_C(c, hA):
                    sl = slice(c * 128, (c + 1) * 128)
                    t1, t1B, t2, t2B = hA
                    rb, rbB = rcur
                    psx, psxB = ps_x.next()
                    S.op("pe", lambda: nc.tensor.matmul(psx[:, 0:128], QT[:, sl], rb[:, 0:128], start=True, stop=True),
                         reads=[QB, rbB], writes=[psxB])
                    S.op("dve", lambda: nc.vector.scalar_tensor_tensor(out=Rst[:, 0:128], in0=Rst[:, 0:128],
                                                                       scalar=rcols[:, hh, 2:3], in1=t2[:, 0:128],
                                                                       op0=ALU.mult, op1=ALU.add),
                         reads=[t2B, kB, RB], writes=[RB])
                    rn, rnB = Rbr.next()
                    S.op("act", lambda: nc.scalar.copy(out=rn[:, 0:128], in_=Rst[:, 0:128]), reads=[RB], writes=[rnB])
                    rcur[0], rcur[1] = rn, rnB
                    nd, ndB = ndr.next()
                    S.op("dve", lambda: nc.vector.scalar_tensor_tensor(out=nd[:, 0:128], in0=psx[:, 0:128],
                                                                       scalar=rcols[:, hh, 1:2], in1=t1[:, 0:128],
                                                                       op0=ALU.mult, op1=ALU.add),
                         reads=[psxB, t1B, kB], writes=[ndB])
                    groupnorm_gate(nd[:, 0:128], ndB, AF.Silu, hh, c)
                hA = ret_A(0)
                for c in range(NCH):
                    hN = ret_A(c + 1) if c + 1 < NCH else None
                    ret_C(c, hA)
                    hA = hN
                P["ystore"](S, hh * 128, Yst, YB)

            S.op("dve", lambda: nc.vector.memset(V1[:, :, 128:129], 1.0), reads=[VB], writes=[VB])
            for hh in range(2):
                S.dma("sp", ldq[:], FMs[16 + hh], reads=[FMsB], writes=[lB])
                S.op("dve", lambda: nc.vector.tensor_scalar(out=f32a[:], in0=ldq[:], scalar1=ccols[:, hh, 3:4],
                                                            scalar2=ccols[:, hh, 4:5], op0=ALU.mult, op1=ALU.add),
                     reads=[lB, kB], writes=[fB])
                for j in range(3):
                    s = 3 - j
                    S.op("dve", lambda j=j, s=s: nc.vector.scalar_tensor_tensor(
                        out=f32a[:, s:SEQ], in0=ldq[:, 0:SEQ - s], scalar=ccols[:, hh, j:j + 1], in1=f32a[:, s:SEQ],
                        op0=ALU.mult, op1=ALU.add), reads=[lB, kB, fB], writes=[fB])
                S.op("act", lambda: nc.scalar.activation(out=ldp[:], in_=f32a[:], func=AF.Silu), reads=[fB], writes=[lB])
                for tb in range(SEQ // 512):
                    tsl = slice(tb * 512, (tb + 1) * 512)
                    for (dst, dstB, wi) in ((QT, QB, hh), (KT, KB_, 2 + hh)):
                        pq, pqB = psq_ring.next()
                        S.op("pe", lambda pq=pq, wi=wi, tsl=tsl: nc.tensor.matmul(pq[:], wqk[:, wi, :], ldp[:, tsl],
                                                                                  start=True, stop=True),
                             reads=[kB, lB], writes=[pqB])
                        S.op("act", lambda pq=pq, dst=dst, tsl=tsl: nc.scalar.copy(out=dst[:, tsl], in_=pq[:]),
                             reads=[pqB], writes=[dstB])
                    pq, pqB = psq_ring.next()
                    S.group("pe", [lambda j=j, pq=pq, tb=tb: nc.tensor.matmul(
                        pq[:, j * 128:(j + 1) * 128], ldp[:, (tb * 4 + j) * 128:(tb * 4 + j + 1) * 128], wqk[:, 2 + hh, :],
                        start=True, stop=True) for j in range(4)], reads=[kB, lB], writes=[pqB])
                    S.op("dve", lambda pq=pq, tb=tb: nc.vector.tensor_copy(
                        out=Ktok[:, tb * 4:(tb + 1) * 4, :], in_=pq[:].rearrange("p (j e) -> p j e", e=128)),
                         reads=[pqB], writes=[KtB])
                load_tok(V1, VB, 512 + hh * 128)
                load_tok(GT, GB, 768 + hh * 128)
                S.op("dve", lambda: nc.vector.memset(Rst[:], 0.0), reads=[RB], writes=[RB])
                rb0, rb0B = Rbr.next()
                S.op("dve", lambda: nc.vector.memset(rb0[:], 0.0), writes=[rb0B])
                rcur = [rb0, rb0B]

                def ml_A(c):
                    sl = slice(c * 128, (c + 1) * 128)

                    def col(q):
                        return colsM[:, c, q * 2 + hh:q * 2 + hh + 1]
                    pss, pssB = ps_s.next()
                    S.op("pe", lambda: nc.tensor.matmul(pss[:], KT[:, sl], QT[:, sl], start=True, stop=True),
                         reads=[KB_, QB], writes=[pssB])
                    PT, PTB = PTr.next()
                    S.op("dve", lambda: nc.vector.scalar_tensor_tensor(out=PT[:], in0=pss[:], scalar=col(0), in1=mskm[:],
                                                                       op0=ALU.mult, op1=ALU.mult),
                         reads=[pssB, kB, colsB], writes=[PTB])
                    pso, psoB = ps_o.next()
                    S.op("pe", lambda: nc.tensor.matmul(pso[:, 0:129], PT[:], V1[:, c, :], start=True, stop=True),
                         reads=[PTB, VB], writes=[psoB])
                    t1, t1B = t1r.next()
                    S.op("act", lambda: nc.scalar.activation(out=t1[:], in_=pso[:, 0:129], func=AF.Copy, scale=col(1)),
                         reads=[psoB, colsB], writes=[t1B])
                    Vz, VzB = Vzr.next()
                    S.op("dve", lambda: nc.vector.tensor_scalar(out=Vz[:], in0=V1[:, c, :], scalar1=col(0), scalar2=ISQ,
                                                                op0=ALU.mult, op1=ALU.mult), reads=[VB, colsB], writes=[VzB])
                    pkv, pkvB = ps_kv.next()
                    S.op("pe", lambda: nc.tensor.matmul(pkv[:, 0:129], Ktok[:, c, :], Vz[:], start=True, stop=True),
                         reads=[KtB, VzB], writes=[pkvB])
                    t2, t2B = t2r.next()
                    S.op("act", lambda: nc.scalar.activation(out=t2[:], in_=pkv[:, 0:129], func=AF.Copy,
                                                             scale=sbM[:, hh, 32 + c:33 + c]), reads=[pkvB, sbB], writes=[t2B])
                    return (t1, t1B, t2, t2B)

                def ml_C(c, hA):
                    sl = slice(c * 128, (c + 1) * 128)

                    def col(q):
                        return colsM[:, c, q * 2 + hh:q * 2 + hh + 1]
                    t1, t1B, t2, t2B = hA
                    rb, rbB = rcur
                    psx, psxB = ps_x.next()
                    S.op("pe", lambda: nc.tensor.matmul(psx[:, 0:129], QT[:, sl], rb[:], start=True, stop=True),
                         reads=[QB, rbB], writes=[psxB])
                    S.op("dve", lambda: nc.vector.scalar_tensor_tensor(out=Rst[:], in0=Rst[:], scalar=sbM[:, hh, c:c + 1],
                                                                       in1=t2[:], op0=ALU.mult, op1=ALU.add),
                         reads=[t2B, sbB, RB], writes=[RB])
                    rn, rnB = Rbr.next()
                    S.op("act", lambda: nc.scalar.copy(out=rn[:], in_=Rst[:]), reads=[RB], writes=[rnB])
                    rcur[0], rcur[1] = rn, rnB
                    nd, ndB = ndr.next()
                    S.op("dve", lambda: nc.vector.scalar_tensor_tensor(out=nd[:, 0:129], in0=psx[:, 0:129], scalar=col(2), in1=t1[:],
                                                                       op0=ALU.mult, op1=ALU.add),
                         reads=[psxB, t1B, colsB], writes=[ndB])
                    S.op("dve", lambda: nc.vector.scalar_tensor_tensor(out=nd[:, 129:130], in0=nd[:, 128:129], scalar=-1.0,
                                                                       in1=nd[:, 128:129], op0=ALU.mult, op1=ALU.max),
                         reads=[ndB], writes=[ndB])
                    S.op("dve", lambda: nc.vector.tensor_tensor(out=nd[:, 130:131], in0=nd[:, 129:130], in1=col(3), op=ALU.max),
                         reads=[ndB, colsB], writes=[ndB])
                    S.op("dve", lambda: nc.vector.reciprocal(out=nd[:, 131:132], in_=nd[:, 130:131]), reads=[ndB], writes=[ndB])
                    S.op("dve", lambda: nc.vector.tensor_scalar(out=nd[:, 0:128], in0=nd[:, 0:128], scalar1=nd[:, 131:132],
                                                                scalar2=None, op0=ALU.mult), reads=[ndB], writes=[ndB])
                    groupnorm_gate(nd[:, 0:128], ndB, AF.Sigmoid, 2 + hh, c)
                hA = ml_A(0)
                for c in range(NCH):
                    hN = ml_A(c + 1) if c + 1 < NCH else None
                    ml_C(c, hA)
                    hA = hN
                P["ystore"](S, 256 + hh * 128, Yst, YB)
            S.barrier()

        with cx.stack() as es:
            QT = cx.sb(es, [128, SEQ], BF16, "fQT")
            KT = cx.sb(es, [128, SEQ], BF16, "fKT")
            V1 = cx.sb(es, [128, NCH, 129], BF16, "fV1")
            Yst = cx.sb(es, [128, NCH, 128], BF16, "fYst")
            mskd = cx.sb(es, [128, 128], F32, "mskd")
            kB = Buf("fconst")
            S.dma("sp", mskd[:], mask_d, writes=[kB])
            QB, KB_, VB, YB = Buf("fQT"), Buf("fKT"), Buf("fV1"), Buf("fYst")
            S.op("dve", lambda: nc.vector.memset(V1[:, :, 128:129], 1.0), writes=[VB])
            ps_s = cx.psring(es, 4, [128, 128], F32, "fps_s")
            ps_o = cx.psring(es, 2, [128, 256], F32, "fps_o")
            PTr = cx.sbring(es, 4, [128, 128], BF16, "fPT")
            rdr = cx.sbring(es, 2, [128, 2], F32, "frd")
            for h in range(4):
                S.dma("sp", QT[:], FMs[h], reads=[FMsB], writes=[QB])
                S.dma("sp", KT[:], FMs[4 + h], reads=[FMsB], writes=[KB_])
                S.dma("sp", V1[:, :, 0:128], TMs[:, 1024 + h * 128:1024 + (h + 1) * 128].rearrange("(c p) e -> p c e", p=128),
                      reads=[TMsB], writes=[VB])
                for qb in range(NCH):
                    qsl = slice(qb * 128, (qb + 1) * 128)
                    pso, psoB = ps_o.next()
                    pend = {}
                    LOOK = 2

                    def issue_s(kb, qsl=qsl, pend=pend):
                        ksl = slice(kb * 128, (kb + 1) * 128)
                        pss, pssB = ps_s.next()
                        S.op("pe", lambda: nc.tensor.matmul(pss[:], KT[:, ksl], QT[:, qsl], start=True, stop=True),
                             reads=[KB_, QB], writes=[pssB])
                        pend[kb] = (pss, pssB)
                    for kb in range(min(LOOK, qb + 1)):
                        issue_s(kb)
                    for kb in range(qb + 1):
                        if kb + LOOK <= qb:
                            issue_s(kb + LOOK)
                        pss, pssB = pend.pop(kb)
                        PT, PTB = PTr.next()
                        S.op("act", lambda: nc.scalar.activation(
                            out=PT[:], in_=pss[:], func=AF.Exp, scale=ISQ, bias=tab[:, h, qb, kb:kb + 1]),
                             reads=[pssB, tabB], writes=[PTB])
                        if kb == qb:
                            S.op("dve", lambda: nc.vector.tensor_tensor(out=PT[:], in0=PT[:], in1=mskd[:], op=ALU.mult),
                                 reads=[PTB, kB], writes=[PTB])
                        S.op("pe", lambda: nc.tensor.matmul(pso[:, 0:129], PT[:], V1[:, kb, :],
                                                            start=(kb == 0), stop=(kb == qb)),
                             reads=[PTB, VB], writes=[psoB])
                    rd, rdB = rdr.next()
                    S.op("dve", lambda rd=rd, pso=pso: nc.vector.reciprocal(out=rd[:, 0:1], in_=pso[:, 128:129]),
                         reads=[psoB], writes=[rdB])
                    S.op("dve", lambda rd=rd, pso=pso, qb=qb: nc.vector.tensor_scalar(
                        out=Yst[:, qb, :], in0=pso[:, 0:128], scalar1=rd[:, 0:1], scalar2=None, op0=ALU.mult),
                         reads=[psoB, rdB], writes=[YB])
                P["ystore"](S, 512 + h * 128, Yst, YB)
            S.barrier()


def _c_decl(nc, sfx=""):
    def din(name, shape, dt=F32):
        return nc.dram_tensor(name + sfx, list(shape), dt, kind="ExternalInput").ap()
    P = {}
    P["adaw_b"] = din("adaw_b", [D, 2 * D])
    P["adaw_c"] = din("adaw_c", [D, 2 * D])
    P["adab_rep"] = din("adab_rep", [128, 2 * D])
    P["adab_col2"] = din("adab_col2", [128, 32])
    P["g2col"] = din("g2col", [128, KC])
    P["w_out"] = din("w_out", [D, D])
    P["w_r"] = din("w_r", [D, 36])
    P["b_r"] = din("b_r", [128, 36])
    P["w1"] = din("w1", [NEXP, D, DEXP])
    P["w3"] = din("w3", [NEXP, D, DEXP])
    P["w2"] = din("w2", [NEXP, DEXP, D])
    return P


def _c_scratch(nc):
    P = {}
    P["x1s"] = nc.dram_tensor("x1s", [TOKC, D], F32, kind="Internal").ap()
    P["h2s"] = nc.dram_tensor("h2s", [KC, 128, TOKC], BF16, kind="Internal").ap()
    P["x1B"], P["h2B"] = Buf("x1s"), Buf("h2s")
    return P


def build_c(last):
    nc = bass.Bass("TRN2", target_bir_lowering=False)
    cx = Ctx(nc)
    S = cx.S

    def din(name, shape, dt=F32):
        return nc.dram_tensor(name, list(shape), dt, kind="ExternalInput").ap()
    P = {}
    P["c_rep"] = din("c_rep", [128, KC, 128])
    P["ident_f"] = din("ident_f", [128, 128])
    P["fg_rep"] = din("fg_rep", [128, D])
    P.update(_c_decl(nc))
    P.update(_c_scratch(nc))
    x = din("x", [TOKC, D])
    yA = din("yA", [TOKC, 1024], BF16)
    yB_ = din("yB", [TOKC, 1024], BF16)
    out = nc.dram_tensor("out", [TOKC, D], F32, kind="ExternalOutput").ap()
    outB = Buf("out")
    P["xsrc"] = lambda t0: x[t0:t0 + 128, :]
    P["xreads"] = []

    def yload(S, tt, yc, ycB, ycB2):
        t0 = tt * 128
        S.dma("sp", yc[:, 0:1024], yA[t0:t0 + 128, :], writes=[ycB])
        S.dma("sp", yc[:, 1024:2048], yB_[t0:t0 + 128, :], reads=[ycB], writes=[ycB2])
    P["yload"] = yload

    def ostore(S, t0, xt, xB):
        S.dma("sp", out[t0:t0 + 128, :], xt[:], reads=[xB], writes=[outB])
    P["ostore"] = ostore
    emit_c(cx, P, last)
    S.close()
    return nc


def emit_c(cx, P, last):
    nc = cx.nc
    S = cx.S
    c_rep, adaw_b, adaw_c, adab_rep, adab_col, g2col = (P["c_rep"], P["adaw_b"], P["adaw_c"], P["adab_rep"],
                                                       P["adab_col2"], P["g2col"])
    w_out, w_r, b_r, w1, w3, w2, ident_f, fg_rep = (P["w_out"], P["w_r"], P["b_r"], P["w1"], P["w3"], P["w2"],
                                                    P["ident_f"], P["fg_rep"])
    x1s, h2s, x1B, h2B = P["x1s"], P["h2s"], P["x1B"], P["h2B"]

    with cx.stack() as es0:
        identf = cx.sb(es0, [128, 128], F32, "identf")
        identb = cx.sb(es0, [128, 128], BF16, "identb")
        epsc = cx.sb(es0, [128, 1], F32, "epsc")
        cB = Buf("consts")
        S.dma("sp", identf[:], ident_f, writes=[cB])
        S.op("dve", lambda: nc.vector.tensor_copy(out=identb[:], in_=identf[:]), reads=[cB], writes=[cB])
        S.op("dve", lambda: nc.vector.memset(epsc[:], EPS), writes=[cB])
        g2t = cx.sb(es0, [128, D], F32, "g2t")
        gBB = Buf("gBt")
        comb = cx.sb(es0, [128, TOKC // 128, NEXP], F32, "comb")
        combB = Buf("comb")

        with cx.stack() as es:
            scale_col = cx.sb(es, [128, KC], F32, "scalecol")
            shift_col = cx.sb(es, [128, KC], F32, "shiftcol")
            modB = Buf("mod")
            cact = cx.sb(es, [128, KC, 128], BF16, "cact")
            cactB = Buf("cact")
            g1t = cx.sb(es, [128, D], F32, "g1t")
            wring = cx.sbring(es, 2, [128, KC, 512], BF16, "wp")
            mmring = cx.psring(es, 2, [128, 512], F32, "mm")
            with cx.stack() as est:
                ctmp = cx.sb(est, [128, KC, 128], F32, "ctmp")
                emit_cact(cx, est, c_rep, cact, cactB, ctmp)
                modps = cx.ps(est, [128, 32], F32, "modps")
                modpsB = Buf("modps")
                modcol = cx.sb(est, [128, 32], F32, "modcol")
                adabc = cx.sb(est, [128, 32], F32, "adabc")
                g2c = cx.sb(est, [128, KC], F32, "g2c")
                aB = Buf("adab")
                S.dma("sp", adabc[:], adab_col, writes=[aB])
                S.dma("sp", g2c[:], g2col, writes=[aB])
                emit_mod_cols(cx, est, adaw_c, cact, cactB, adabc, modcol, modB, 32, wring, modps, modpsB)
                S.op("dve", lambda: nc.vector.tensor_copy(out=shift_col[:], in_=modcol[:, 0:KC]), reads=[modB], writes=[modB])
                S.op("dve", lambda: nc.vector.scalar_tensor_tensor(out=scale_col[:], in0=modcol[:, KC:2 * KC], scalar=1.0,
                                                                   in1=g2c[:], op0=ALU.add, op1=ALU.mult),
                     reads=[modB, aB], writes=[modB])
                abr = cx.sb(est, [128, 2 * D], F32, "abr")
                S.dma("sp", abr[:], adab_rep, writes=[aB])
                for pc in range(8):
                    wt, wb = wring.next()
                    S.dma("pool", wt[:], adaw_b[:, pc * 512:(pc + 1) * 512].rearrange("(k p) n -> p k n", p=128), writes=[wb])
                    mm, mmB = mmring.next()
                    S.group("pe", [lambda k=k, mm=mm, wt=wt: nc.tensor.matmul(mm[:], cact[:, k, :], wt[:, k, :],
                                                                           start=(k == 0), stop=(k == KC - 1))
                                   for k in range(KC)], reads=[wb, cactB], writes=[mmB])
                    S.op("dve", lambda pc=pc, mm=mm: nc.vector.tensor_tensor(
                        out=(g1t if pc < 4 else g2t)[:, (pc % 4) * 512:(pc % 4 + 1) * 512], in0=mm[:],
                        in1=abr[:, pc * 512:(pc + 1) * 512], op=ALU.add), reads=[mmB, aB], writes=[gBB])
                S.barrier()

            wo = cx.sb(es, [128, KC, D], BF16, "wo")
            woB = Buf("wo")
            for k4 in range(4):
                S.dma("pool", wo[:, k4 * 4:(k4 + 1) * 4, :],
                      w_out[k4 * 512:(k4 + 1) * 512, :].rearrange("(k p) n -> p k n", p=128), writes=[woB])
            wr = cx.sb(es, [128, KC, 36], F32, "wr")
            br = cx.sb(es, [128, 36], F32, "br")
            wrB = Buf("wr")
            S.dma("sp", wr[:], w_r.rearrange("(k p) n -> p k n", p=128), writes=[wrB])
            S.dma("sp", br[:], b_r, writes=[wrB])
            ycr = cx.sbring(es, 2, [128, D], BF16, "yc")
            if "c1_hook" in P:
                P["c1_hook"](cx, es)
            yTr = cx.sbring(es, 2, [128, KC, 128], BF16, "yT")
            xring = cx.sbring(es, 2, [128, D], F32, "xt")
            xnring = cx.sbring(es, 1, [128, D], F32, "xn")
            junk = cx.sb(es, [128, D], BF16, "junk")
            junkB = Buf("junk")
            ssring = cx.sbring(es, 2, [128, 4], F32, "ss")
            tpb = cx.psring(es, 1, [128, KC, 128], BF16, "tpb")
            tpf = cx.psring(es, 2, [128, 4, 128], F32, "tpf")
            h2fr = cx.sbring(es, 1, [128, KC, 128], F32, "h2f")
            h2br = cx.sbring(es, 2, [128, KC, 128], BF16, "h2b")
            tmr = cx.sbring(es, 2, [128, 512], F32, "tm")
            psr = cx.psring(es, 1, [128, 64], F32, "psr")
            rt = cx.sbring(es, 2, [128, 96], F32, "rt")
            pref = {}

            def c1_load(tt_):
                yc_, ycB_ = ycr.next()
                ycB2_ = Buf("yc2")
                P["yload"](S, tt_, yc_, ycB_, ycB2_)
                xt_, xB_ = xring.next()
                S.dma("sp", xt_[:], P["xsrc"](tt_ * 128), reads=P["xreads"], writes=[xB_])
                pref[tt_] = (yc_, ycB_, ycB2_, xt_, xB_)
            c1_load(0)
            for tt in range(TOKC // 128):
                t0 = tt * 128
                yc, ycB, ycB2, xt, xB = pref.pop(tt)
                tp, tpB = tpb.next()
                S.group("pe", [lambda k=k: nc.tensor.transpose(tp[:, k, :], yc[:, k * 128:(k + 1) * 128], identb[:])
                               for k in range(KC)], reads=[ycB, ycB2, cB], writes=[tpB])
                yT, yTB = yTr.next()
                S.op("act", lambda: nc.scalar.copy(out=yT[:, 0:8, :], in_=tp[:, 0:8, :]), reads=[tpB], writes=[yTB])
                yTB2 = Buf("yT2")
                S.op("dve", lambda: nc.vector.tensor_copy(out=yT[:, 8:16, :], in_=tp[:, 8:16, :]), reads=[tpB, yTB], writes=[yTB2])
                if tt + 1 < TOKC // 128:
                    c1_load(tt + 1)
                for cb in range(4):
                    csl = slice(cb * 512, (cb + 1) * 512)
                    mm, mmB = mmring.next()
                    S.group("pe", [lambda k=k, mm=mm: nc.tensor.matmul(mm[:], yT[:, k, :], wo[:, k, csl],
                                                                        start=(k == 0), stop=(k == KC - 1))
                                   for k in range(KC)], reads=[yTB, yTB2, woB], writes=[mmB])
                    tm, tmB = tmr.next()
                    S.op("dve", lambda: nc.vector.tensor_tensor(out=tm[:], in0=mm[:], in1=g1t[:, csl], op=ALU.mult),
                         reads=[mmB, gBB], writes=[tmB])
                    S.op("dve", lambda: nc.vector.tensor_tensor(out=xt[:, csl], in0=xt[:, csl], in1=tm[:], op=ALU.add),
                         reads=[tmB, xB], writes=[xB])
                S.dma("sp", x1s[t0:t0 + 128, :], xt[:], reads=[xB], writes=[x1B])
                ss, ssB = ssring.next()
                emit_rms_stats(cx, xt, xB, junk, junkB, ss, ssB, epsc)
                xn, xnB = xnring.next()
                S.op("dve", lambda: nc.vector.tensor_scalar(out=xn[:], in0=xt[:], scalar1=ss[:, 2:3], scalar2=None,
                                                            op0=ALU.mult), reads=[xB, ssB], writes=[xnB])
                h2f, h2fB = h2fr.next()
                for k4 in range(4):
                    tf, tfB = tpf.next()
                    S.group("pe", [lambda j=j, tf=tf, k4=k4: nc.tensor.transpose(
                        tf[:, j, :], xn[:, (k4 * 4 + j) * 128:(k4 * 4 + j + 1) * 128], identf[:]) for j in range(4)],
                            reads=[xnB, cB], writes=[tfB])
                    for j in range(4):
                        k = k4 * 4 + j
                        S.op("act", lambda j=j, k=k, tf=tf: nc.scalar.activation(
                            out=h2f[:, k, :], in_=tf[:, j, :], func=AF.Identity, scale=scale_col[:, k:k + 1],
                            bias=shift_col[:, k:k + 1]), reads=[tfB, modB], writes=[h2fB])
                h2b, h2bB = h2br.next()
                S.op("dve", lambda: nc.vector.tensor_copy(out=h2b[:], in_=h2f[:]), reads=[h2fB], writes=[h2bB])
                S.dma("sp", h2s[:, :, t0:t0 + 128].rearrange("k p t -> p k t"), h2b[:], reads=[h2bB], writes=[h2B])
                pr, prB = psr.next()
                S.group("pe", [lambda k=k: nc.tensor.matmul(pr[:, 0:36], h2f[:, k, :], wr[:, k, :],
                                                            start=(k == 0), stop=(k == KC - 1)) for k in range(KC)],
                        reads=[h2fB, wrB], writes=[prB])
                r, rB_ = rt.next()
                V = nc.vector

                def dv(fn):
                    S.op("dve", fn, reads=[rB_], writes=[rB_])
                S.op("dve", lambda: V.tensor_tensor(out=r[:, 0:36], in0=pr[:, 0:36], in1=br[:], op=ALU.add),
                     reads=[prB, wrB], writes=[rB_])
                dv(lambda: V.reduce_max(out=r[:, 40:41], in_=r[:, 0:4], axis=AX.X))
                dv(lambda: V.tensor_scalar(out=r[:, 36:40], in0=r[:, 0:4], scalar1=r[:, 40:41], scalar2=None, op0=ALU.is_equal))
                dv(lambda: V.tensor_scalar(out=r[:, 88:92], in0=r[:, 0:4], scalar1=r[:, 40:41], scalar2=None, op0=ALU.subtract))
                S.op("act", lambda: nc.scalar.activation(out=r[:, 88:92], in_=r[:, 88:92], func=AF.Exp, accum_out=r[:, 41:42]),
                     reads=[rB_], writes=[rB_])
                dv(lambda: V.reciprocal(out=r[:, 42:43], in_=r[:, 41:42]))
                dv(lambda: V.tensor_scalar(out=r[:, 44:52], in0=r[:, 4:12], scalar1=r[:, 36:37], scalar2=None, op0=ALU.mult))
                for gg in range(1, 4):
                    dv(lambda gg=gg: V.scalar_tensor_tensor(out=r[:, 44:52], in0=r[:, 4 + gg * 8:12 + gg * 8],
                                                            scalar=r[:, 36 + gg:37 + gg], in1=r[:, 44:52],
                                                            op0=ALU.mult, op1=ALU.add))
                dv(lambda: V.reduce_max(out=r[:, 76:77], in_=r[:, 44:52], axis=AX.X))
                dv(lambda: V.tensor_scalar(out=r[:, 52:60], in0=r[:, 44:52], scalar1=r[:, 76:77], scalar2=None, op0=ALU.is_equal))
                dv(lambda: V.scalar_tensor_tensor(out=r[:, 60:68], in0=r[:, 52:60], scalar=NEG, in1=r[:, 44:52],
                                                  op0=ALU.mult, op1=ALU.add))
                dv(lambda: V.reduce_max(out=r[:, 77:78], in_=r[:, 60:68], axis=AX.X))
                dv(lambda: V.tensor_scalar(out=r[:, 68:76], in0=r[:, 60:68], scalar1=r[:, 77:78], scalar2=None, op0=ALU.is_equal))
                dv(lambda: V.tensor_tensor(out=r[:, 78:79], in0=r[:, 76:77], in1=r[:, 77:78], op=ALU.subtract))
                S.op("act", lambda: nc.scalar.activation(out=r[:, 78:79], in_=r[:, 78:79], func=AF.Sigmoid),
                     reads=[rB_], writes=[rB_])
                dv(lambda: V.tensor_tensor(out=r[:, 78:79], in0=r[:, 78:79], in1=r[:, 42:43], op=ALU.mult))
                dv(lambda: V.tensor_tensor(out=r[:, 79:80], in0=r[:, 42:43], in1=r[:, 78:79], op=ALU.subtract))
                dv(lambda: V.tensor_scalar(out=r[:, 80:88], in0=r[:, 52:60], scalar1=r[:, 78:79], scalar2=None, op0=ALU.mult))
                dv(lambda: V.scalar_tensor_tensor(out=r[:, 80:88], in0=r[:, 68:76], scalar=r[:, 79:80], in1=r[:, 80:88],
                                                  op0=ALU.mult, op1=ALU.add))
                for gg in range(4):
                    S.op("dve", lambda gg=gg: V.tensor_scalar(out=comb[:, tt, gg * 8:(gg + 1) * 8], in0=r[:, 80:88],
                                                              scalar1=r[:, 36 + gg:37 + gg], scalar2=None, op0=ALU.mult),
                         reads=[rB_], writes=[combB])
            S.barrier()

        with cx.stack() as es:
            TB = 1024
            acc = cx.sb(es, [128, TB // 128, D], F32, "acc")
            accB = [Buf("acc%d" % i) for i in range(TB // 128)]
            hT = cx.sb(es, [128, KC, TB], BF16, "hT")
            hB = Buf("hT")
            wring = cx.sbring(es, 4, [128, 8192], BF16, "wexp")
            G = cx.sbring(es, 2, [128, 4, 512], BF16, "G")
            sil = cx.sbring(es, 2, [128, 512], F32, "sil")
            ps13 = cx.psring(es, 4, [128, 512], F32, "ps13")
            psy = cx.psring(es, 4, [128, 512], F32, "psy")
            xring = cx.sbring(es, 1, [128, D], F32, "xt2")
            fgr = None
            if last:
                fgr = cx.sb(es, [128, D], F32, "fgr")
                fgB = Buf("fgr")
                S.dma("sp", fgr[:], fg_rep, writes=[fgB])
                junk = cx.sb(es, [128, D], BF16, "junk2")
                junkB = Buf("junk2")
                ssring = cx.sbring(es, 2, [128, 4], F32, "ss2")
            for ps_ in range(TOKC // TB):
                tok0 = ps_ * TB
                for k4 in range(4):
                    S.dma("sp", hT[:, k4 * 4:(k4 + 1) * 4, :],
                          h2s[k4 * 4:(k4 + 1) * 4, :, tok0:tok0 + TB].rearrange("k p t -> p k t"),
                          reads=[h2B], writes=[hB] if k4 == 0 else [Buf("hTx%d" % k4)])
                S.barrier(["pe"])
                for e in range(NEXP):
                    w1t, w1B = wring.next()
                    S.dma("pool", w1t[:].rearrange("p (k n) -> p k n", n=DEXP), w1[e].rearrange("(k p) n -> p k n", p=128),
                          writes=[w1B])
                    w3t, w3B = wring.next()
                    S.dma("pool", w3t[:].rearrange("p (k n) -> p k n", n=DEXP), w3[e].rearrange("(k p) n -> p k n", p=128),
                          writes=[w3B])
                    w2t, w2B = wring.next()
                    S.dma("pool", w2t[:].rearrange("p (k n) -> p k n", n=D), w2[e].rearrange("(k p) n -> p k n", p=128),
                          writes=[w2B])
                    w1v = w1t[:].rearrange("p (k n) -> p k n", n=DEXP)
                    w3v = w3t[:].rearrange("p (k n) -> p k n", n=DEXP)
                    w2v = w2t[:].rearrange("p (k n) -> p k n", n=D)
                    for half in range(TB // 512):
                        tsl = slice(half * 512, (half + 1) * 512)
                        Gt, GB = G.next()
                        for hc in range(4):
                            hsl = slice(hc * 128, (hc + 1) * 128)
                            p1, p1B = ps13.next()
                            S.group("pe", [lambda k=k, p1=p1: nc.tensor.matmul(p1[:], w1v[:, k, hsl], hT[:, k, tsl],
                                                                                start=(k == 0), stop=(k == KC - 1))
                                           for k in range(KC)], reads=[w1B, hB], writes=[p1B])
                            p3, p3B = ps13.next()
                            S.group("pe", [lambda k=k, p3=p3: nc.tensor.matmul(p3[:], w3v[:, k, hsl], hT[:, k, tsl],
                                                                                start=(k == 0), stop=(k == KC - 1))
                                           for k in range(KC)], reads=[w3B, hB], writes=[p3B])
                            sl_, slB = sil.next()
                            S.op("act", lambda: nc.scalar.activation(out=sl_[:], in_=p1[:], func=AF.Silu), reads=[p1B], writes=[slB])
                            S.op("dve", lambda: nc.vector.tensor_tensor(out=Gt[:, hc, :], in0=p3[:], in1=sl_[:], op=ALU.mult),
                                 reads=[p3B, slB], writes=[GB])
                        for t4 in range(4):
                            tt = half * 4 + t4
                            gt = ps_ * (TB // 128) + tt
                            for cb in range(4):
                                csl = slice(cb * 512, (cb + 1) * 512)
                                py, pyB = psy.next()
                                S.group("pe", [lambda hc=hc, py=py: nc.tensor.matmul(
                                    py[:], Gt[:, hc, t4 * 128:(t4 + 1) * 128], w2v[:, hc, csl],
                                    start=(hc == 0), stop=(hc == 3)) for hc in range(4)], reads=[GB, w2B], writes=[pyB])
                                if e == 0:
                                    S.op("dve", lambda: nc.vector.tensor_scalar(out=acc[:, tt, csl], in0=py[:],
                                                                                scalar1=comb[:, gt, e:e + 1], scalar2=None,
                                                                                op0=ALU.mult),
                                         reads=[pyB, combB], writes=[accB[tt]])
                                else:
                                    S.op("dve", lambda: nc.vector.scalar_tensor_tensor(
                                        out=acc[:, tt, csl], in0=py[:], scalar=comb[:, gt, e:e + 1], in1=acc[:, tt, csl],
                                        op0=ALU.mult, op1=ALU.add), reads=[pyB, combB, accB[tt]], writes=[accB[tt]])
                for tt in range(TB // 128):
                    t0 = tok0 + tt * 128
                    xt, xB = xring.next()
                    S.dma("sp", xt[:], x1s[t0:t0 + 128, :], reads=[x1B], writes=[xB])
                    S.op("dve", lambda: nc.vector.tensor_tensor(out=acc[:, tt, :], in0=acc[:, tt, :], in1=g2t[:], op=ALU.mult),
                         reads=[accB[tt], gBB], writes=[accB[tt]])
                    S.op("dve", lambda: nc.vector.tensor_tensor(out=xt[:], in0=xt[:], in1=acc[:, tt, :], op=ALU.add),
                         reads=[accB[tt], xB], writes=[xB])
                    if last:
                        ss, ssB = ssring.next()
                        emit_rms_stats(cx, xt, xB, junk, junkB, ss, ssB, epsc)
                        S.op("dve", lambda: nc.vector.scalar_tensor_tensor(out=xt[:], in0=xt[:], scalar=ss[:, 2:3], in1=fgr[:],
                                                                           op0=ALU.mult, op1=ALU.mult),
                             reads=[xB, ssB, fgB], writes=[xB])
                    P["ostore"](S, t0, xt, xB)
                S.barrier()


OFF = dict(rq=0, rk=512, rv=1024, rg=1536, mx=2048, mv=2560, mo=3072, mi=3584, mf=3588,
           fq=3592, fk=4616, fv=5640, ff=6664)
_CONST = {}
_PROG = {}


def _consts():
    if _CONST:
        return _CONST
    half = 64
    inv = (10000.0 ** (-np.arange(half, dtype=np.float32) / np.float32(half))).astype(np.float32)
    pos = np.arange(SEQ, dtype=np.float32)
    ang = (pos[:, None] * inv[None, :]).astype(np.float32)
    cos = np.cos(ang).astype(np.float32).T
    sin = np.sin(ang).astype(np.float32).T
    _CONST["rotC"] = np.ascontiguousarray(np.concatenate([cos, cos], 0))
    _CONST["rotS"] = np.ascontiguousarray(np.concatenate([-sin, sin], 0))
    _CONST["ident_f"] = np.eye(128, dtype=np.float32)
    j = np.arange(128)[:, None]
    i = np.arange(128)[None, :]
    _CONST["mask_m"] = np.where(j <= i, ISQ, 0.0).astype(np.float32)
    _CONST["mask_d"] = np.where(j <= i, 1.0, 0.0).astype(np.float32)
    rows = np.zeros((4, 3, SEQ), np.float32)
    rows[:, 0, :] = 1.0
    rows[:, 1, :] = 1.0
    rows[:, 1, ::128] = 0.0
    rows[:, 2, ::128] = NEG
    _CONST["rows_c"] = rows
    oh = np.zeros((4, 4, 128), np.float32)
    for h in range(4):
        oh[h, h, :] = 1.0
    _CONST["onehot"] = oh
    DT = np.zeros((4, 128, 128), np.float64)
    cols = np.zeros((4, 128, 3), np.float64)
    for h in range(4):
        lg = np.log1p(-(2.0 ** (-5.0 - h)))
        diff = (i - j).astype(np.float64)
        DT[h] = np.where(diff >= 0, np.exp(np.maximum(diff, 0) * lg), 0.0) * ISQ
        p = np.arange(128, dtype=np.float64)
        cols[h, :, 0] = np.exp((127 - p) * lg) * ISQ
        cols[h, :, 1] = np.exp((p + 1) * lg)
        cols[h, :, 2] = np.exp(128 * lg)
    _CONST["DT"] = DT.astype(np.float32)
    _CONST["rcols"] = cols.astype(np.float32)
    return _CONST


def _col_layout(v, n):
    return np.ascontiguousarray(v.reshape(n, 128).T)


def _prog(kind):
    if kind not in _PROG:
        _PROG[kind] = build_ab() if kind == "ab" else build_c(kind == "c_last")
    return _PROG[kind]


def _ab_inputs(l, b, hh, xb, inp):
    K = _consts()
    w_in = inp["w_in"][l]
    perm = np.concatenate([np.arange(64, 128), np.arange(0, 64)])
    cols = []
    for i in range(4):
        cols.append(np.arange(128) + OFF["fq"] + (hh * 4 + i) * 128)
    for i in range(4):
        cols.append(np.arange(128) + OFF["fk"] + (hh * 4 + i) * 128)
    for i in range(2):
        cols.append(np.arange(128) + OFF["rq"] + (hh * 2 + i) * 128)
    for i in range(2):
        cols.append(np.arange(128) + OFF["rk"] + (hh * 2 + i) * 128)
    for i in range(2):
        cols.append(perm + OFF["rq"] + (hh * 2 + i) * 128)
    for i in range(2):
        cols.append(perm + OFF["rk"] + (hh * 2 + i) * 128)
    for i in range(2):
        cols.append(np.arange(128) + OFF["mx"] + (hh * 2 + i) * 128)
    fm_cols = np.concatenate(cols)
    tm_cols = np.concatenate([np.arange(256) + OFF["rv"] + hh * 256, np.arange(256) + OFF["rg"] + hh * 256,
                              np.arange(256) + OFF["mv"] + hh * 256, np.arange(256) + OFF["mo"] + hh * 256,
                              np.arange(512) + OFF["fv"] + hh * 512])
    g_cols = np.concatenate([np.arange(2) + OFF["mi"] + hh * 2, np.arange(2) + OFF["mf"] + hh * 2,
                             np.arange(4) + OFF["ff"] + hh * 4])
    c = inp["c"][b]
    gate_b = np.zeros((4, 3), np.float32)
    gate_b[0:2, 0] = inp["mlstm_i_b"][l][hh * 2:hh * 2 + 2]
    gate_b[0:2, 1] = inp["mlstm_f_b"][l][hh * 2:hh * 2 + 2]
    gate_b[:, 2] = inp["fox_f_b"][l][hh * 4:hh * 4 + 4]
    gn = np.concatenate([inp["ret_gn_g"][l][hh * 256:(hh + 1) * 256], inp["mlstm_gn_g"][l][hh * 256:(hh + 1) * 256]])
    conv = np.zeros((128, 2, 5), np.float32)
    for i in range(2):
        sl = slice((hh * 2 + i) * 128, (hh * 2 + i + 1) * 128)
        conv[:, i, 0:4] = inp["mlstm_conv_w"][l][:, sl].T
        conv[:, i, 4] = inp["mlstm_conv_b"][l][sl]
    return {
        "x": xb,
        "c_rep": np.ascontiguousarray(np.broadcast_to(_col_layout(c, KC)[:, :, None], (128, KC, 128))),
        "adaw": np.ascontiguousarray(inp["ada_w"][l][:, 0:2 * D]),
        "adab_col": _col_layout(inp["ada_b"][l][0:2 * D], 32),
        "g1col": _col_layout(inp["norm1_g"][l], KC),
        "w_fm": np.ascontiguousarray(w_in[:, fm_cols]),
        "w_tm": np.ascontiguousarray(w_in[:, tm_cols]),
        "w_g": np.ascontiguousarray(w_in[:, g_cols]),
        "ident_f": K["ident_f"], "rotC": K["rotC"], "rotS": K["rotS"],
        "ret_DT": np.ascontiguousarray(K["DT"][hh * 2:hh * 2 + 2].transpose(1, 0, 2)),
        "ret_cols": np.ascontiguousarray(K["rcols"][hh * 2:hh * 2 + 2].transpose(1, 0, 2)),
        "gn_rep": np.ascontiguousarray(np.broadcast_to(gn.reshape(1, 4, 128), (128, 4, 128))),
        "conv_cols": conv,
        "wq": np.ascontiguousarray(inp["mlstm_wq"][l][hh * 2:hh * 2 + 2]),
        "wk": np.ascontiguousarray(inp["mlstm_wk"][l][hh * 2:hh * 2 + 2]),
        "gate_b": gate_b, "rows_c": K["rows_c"], "mask_m": K["mask_m"], "mask_d": K["mask_d"], "onehot": K["onehot"],
    }


def _c_inputs(l, b, th, xb, ys, inp, shared):
    K = _consts()
    rows = slice(th * TOKC, (th + 1) * TOKC)
    c = inp["c"][b]
    d = {
        "x": np.ascontiguousarray(xb[rows]),
        "yA": np.ascontiguousarray(ys[0][rows]),
        "yB": np.ascontiguousarray(ys[1][rows]),
        "c_rep": np.ascontiguousarray(np.broadcast_to(_col_layout(c, KC)[:, :, None], (128, KC, 128))),
        "ident_f": K["ident_f"],
    }
    d.update(shared)
    return d


def _c_shared(l, inp):
    ada_w = inp["ada_w"][l]
    ada_b = inp["ada_b"][l]
    gsel = np.concatenate([np.arange(2 * D, 3 * D), np.arange(5 * D, 6 * D)])
    wo = inp["w_out"][l]
    ro = np.concatenate([np.arange(0, 256), np.arange(512, 768), np.arange(1024, 1536),
                         np.arange(256, 512), np.arange(768, 1024), np.arange(1536, 2048)])
    br = np.concatenate([inp["router_group_b"][l], inp["router_expert_b"][l]])
    return {
        "adaw_b": np.ascontiguousarray(ada_w[:, gsel]),
        "adaw_c": np.ascontiguousarray(ada_w[:, 3 * D:5 * D]),
        "adab_rep": np.ascontiguousarray(np.broadcast_to(ada_b[gsel][None, :], (128, 2 * D))),
        "adab_col2": _col_layout(ada_b[3 * D:5 * D], 32),
        "g2col": _col_layout(inp["norm2_g"][l], KC),
        "w_out": np.ascontiguousarray(wo[ro, :]),
        "w_r": np.ascontiguousarray(np.concatenate([inp["router_group_w"][l], inp["router_expert_w"][l]], axis=1)),
        "b_r": np.ascontiguousarray(np.broadcast_to(br[None, :], (128, 36))),
        "w1": inp["moe_w1"][l], "w3": inp["moe_w3"][l], "w2": inp["moe_w2"][l],
        "fg_rep": np.ascontiguousarray(np.broadcast_to(inp["final_g"][None, :], (128, D))),
    }


AB_LAYER_KEYS = ("adaw", "adab_col", "g1col", "w_fm", "w_tm", "w_g", "gn_rep", "conv_cols", "wq", "wk", "gate_b")
C_LAYER_KEYS = ("adaw_b", "adaw_c", "adab_rep", "adab_col2", "g2col", "w_out", "w_r", "b_r", "w1", "w3", "w2")
AB_CONST_KEYS = ("c_rep", "ident_f", "rotC", "rotS", "ret_DT", "ret_cols", "rows_c", "mask_m", "mask_d", "onehot")
GROUPS = [[0, 1], [2, 3], [4, 5], [6, 7]]
NYC = 4
NXC = 8


def build_fused(depth=DEPTH):
    nc = bass.Bass("TRN2", target_bir_lowering=False)
    cx = Ctx(nc)
    S = cx.S

    def din(name, shape, dt=F32):
        return nc.dram_tensor(name, list(shape), dt, kind="ExternalInput").ap()

    def dint(name, shape, dt):
        return nc.dram_tensor(name, list(shape), dt, kind="Internal").ap()
    consts = _ab_const_decl(nc)
    fg_rep = din("fg_rep", [128, D])
    msel_d = din("msel", [128, 2])
    x_all0 = din("x_all0", [SEQ, D])
    x_half0 = din("x_half0", [TOKC, D])
    out = nc.dram_tensor("out", [TOKC, D], F32, kind="ExternalOutput").ap()
    outB = Buf("out")
    abs_ = _ab_scratch(nc)
    cs_ = _c_scratch(nc)
    ysend = [dint("ysend%d" % c, [SEQ // NYC, 1024], BF16) for c in range(NYC)]
    yall = [dint("yall%d" % c, [2 * SEQ // NYC, 1024], BF16) for c in range(NYC)]
    xloc = [dint("xloc%d" % c, [TOKC // NXC, D], F32) for c in range(NXC)]
    xall = [dint("xall%d" % c, [2 * TOKC // NXC, D], F32) for c in range(NXC)]
    ysB, yaB, xlB, xaB = Buf("ysend"), Buf("yall"), Buf("xloc"), Buf("xall")
    YR = SEQ // NYC
    XR = TOKC // NXC
    ccsems = []

    def gather(srcs, dsts):
        S.barrier()
        S.new_epoch()
        toks = []
        for s_, d_ in zip(srcs, dsts):
            g = nc.semaphore("cc%d" % len(ccsems))
            sem = g.__enter__()
            ccsems.append(g)
            nc.gpsimd.collective_compute("AllGather", ALU.bypass, replica_groups=GROUPS, ins=[s_], outs=[d_]).then_inc(sem)
            toks.append((sem, 1))
        for e in S.eng:
            for t in toks:
                S._wait(e, t)

    with cx.stack() as esg:
        msel = cx.sb(esg, [128, 2], F32, "msel")
        mB = Buf("msel")
        S.dma("sp", msel[:], msel_d, writes=[mB])
        for l in range(depth):
            P = dict(consts)
            P.update(_ab_decl(nc, "_%d" % l))
            P.update(abs_)
            if l == 0:
                P["xsrc"] = lambda t0: x_all0[t0:t0 + 128, :]
                P["xreads"] = []
            else:
                def xsrc_ab(t0):
                    r, rem = t0 // TOKC, t0 % TOKC
                    c, i0 = rem // XR, rem % XR
                    return xall[c][r * XR + i0:r * XR + i0 + 128, :]
                P["xsrc"] = xsrc_ab
                P["xreads"] = [xaB]

            def ystore(S_, col0, Yst, YB):
                for c in range(NYC):
                    S_.dma("sp", ysend[c][:, col0:col0 + 128].rearrange("(c p) e -> p c e", p=128),
                           Yst[:, c * (YR // 128):(c + 1) * (YR // 128), :], reads=[YB], writes=[ysB])
            P["ystore"] = ystore
            emit_ab(cx, P)
            gather(ysend, yall)

            Pc = {"c_rep": consts["c_rep"], "ident_f": consts["ident_f"], "fg_rep": fg_rep}
            Pc.update(_c_decl(nc, "_%d" % l))
            Pc.update(cs_)
            if l == 0:
                Pc["xsrc"] = lambda t0: x_half0[t0:t0 + 128, :]
                Pc["xreads"] = []
            else:
                Pc["xsrc"] = lambda t0: xloc[t0 // XR][t0 % XR:t0 % XR + 128, :]
                Pc["xreads"] = [xlB]
            cand = {}

            def c1_hook(cx_, es_, cand=cand):
                cand["r"] = cx_.sbring(es_, 2, [128, 2, D], BF16, "cand")

            def yload(S_, tt, yc, ycB, ycB2, cand=cand):
                ct, cB_ = cand["r"].next()
                xb = cand.setdefault(cB_.name, [Buf("c1"), Buf("c2"), Buf("c3")])
                bl = [cB_] + xb
                for hf in range(2):
                    t = hf * TOKC + tt * 128
                    c, i0 = t // YR, t % YR
                    S_.dma("sp", ct[:, hf, 0:1024], yall[c][i0:i0 + 128, :], reads=[yaB], writes=[bl[hf * 2]])
                    S_.dma("sp", ct[:, hf, 1024:2048], yall[c][YR + i0:YR + i0 + 128, :], reads=[yaB], writes=[bl[hf * 2 + 1]])
                S_.op("dve", lambda: nc.vector.tensor_scalar(out=yc[:], in0=ct[:, 0, :], scalar1=msel[:, 0:1], scalar2=None,
                                                             op0=ALU.mult), reads=bl + [mB], writes=[ycB])
                S_.op("dve", lambda: nc.vector.scalar_tensor_tensor(out=yc[:], in0=ct[:, 1, :], scalar=msel[:, 1:2], in1=yc[:],
                                                                    op0=ALU.mult, op1=ALU.add), reads=bl + [mB, ycB], writes=[ycB])
            Pc["c1_hook"] = c1_hook
            Pc["yload"] = yload
            if l == depth - 1:
                def ostore(S_, t0, xt, xB):
                    S_.dma("sp", out[t0:t0 + 128, :], xt[:], reads=[xB], writes=[outB])
            else:
                def ostore(S_, t0, xt, xB):
                    S_.dma("sp", xloc[t0 // XR][t0 % XR:t0 % XR + 128, :], xt[:], reads=[xB], writes=[xlB])
            Pc["ostore"] = ostore
            emit_c(cx, Pc, l == depth - 1)
            if l < depth - 1:
                gather(xloc, xall)
        S.barrier()
    for g in reversed(ccsems):
        g.__exit__(None, None, None)
    S.close()
    return nc


def kernel(**inputs):
    inp = {k: np.asarray(v) for k, v in inputs.items()}
    x = np.ascontiguousarray(inp["x"], dtype=np.float32)
    cores = list(range(8))
    if "fused" not in _PROG:
        _PROG["fused"] = build_fused()
    K = _consts()
    shared = [_c_shared(l, inp) for l in range(DEPTH)]
    in_maps = []
    for r in cores:
        b, hh = r // 2, r % 2
        m = {}
        for l in range(DEPTH):
            d = _ab_inputs(l, b, hh, None, inp)
            if l == 0:
                for k in AB_CONST_KEYS:
                    m[k] = d[k]
            for k in AB_LAYER_KEYS:
                m["%s_%d" % (k, l)] = d[k]
            for k in C_LAYER_KEYS:
                m["%s_%d" % (k, l)] = shared[l][k]
        m["fg_rep"] = shared[0]["fg_rep"]
        ms = np.zeros((128, 2), np.float32)
        ms[:, hh] = 1.0
        m["msel"] = ms
        m["x_all0"] = x[b]
        m["x_half0"] = np.ascontiguousarray(x[b, hh * TOKC:(hh + 1) * TOKC])
        in_maps.append(m)
    res = run_bass_kernel_spmd(_PROG["fused"], in_maps, core_ids=cores)
    outp = np.empty_like(x)
    for r in cores:
        outp[r // 2, (r % 2) * TOKC:(r % 2 + 1) * TOKC] = np.asarray(res.results[r]["out"])
    return outp


def kernel_unfused(**inputs):
    inp = {k: np.asarray(v) for k, v in inputs.items()}
    xcur = np.ascontiguousarray(inp["x"], dtype=np.float32)
    cores = list(range(8))
    for l in range(DEPTH):
        in_maps = [_ab_inputs(l, r // 2, r % 2, xcur[r // 2], inp) for r in cores]
        res = run_bass_kernel_spmd(_prog("ab"), in_maps, core_ids=cores)
        ys = [np.asarray(res.results[r]["y"]) for r in cores]
        shared = _c_shared(l, inp)
        in_maps = [_c_inputs(l, r // 2, r % 2, xcur[r // 2], (ys[(r // 2) * 2], ys[(r // 2) * 2 + 1]), inp, shared)
                   for r in cores]
        res = run_bass_kernel_spmd(_prog("c_last" if l == DEPTH - 1 else "c"), in_maps, core_ids=cores)
        xn = np.empty_like(xcur)
        for r in cores:
            xn[r // 2, (r % 2) * TOKC:(r % 2 + 1) * TOKC] = np.asarray(res.results[r]["out"])
        xcur = xn
    return xcur
```
